# Optimizing a Trainium2 kernel written in Bass

```python
import math
import jax
import jax.numpy as jnp
from jax import lax
import numpy as np

D_MODEL = 1024
BATCH = 4
SEQ = 8192
DEPTH = 2

GRID_W = 64
CTX_LEN = 256
N_MIXERS = 2
EPS = 1e-6
N_MOD = 6
ATT_HEADS = 16
ATT_KV_HEADS = 4
HEAD_DIM = 64
ATT_Q_W = ATT_HEADS * HEAD_DIM
ATT_KV_W = ATT_KV_HEADS * HEAD_DIM
ATT_PROJ_W = ATT_Q_W + 2 * ATT_KV_W
WINDOW = 128
ATT_BLOCK = 128
ROPE_BASE = 10000.0
ROPE_FREQS = HEAD_DIM // 4
DN_HEADS = 8
DN_DK = 128
DN_DV = 128
DN_KW = DN_HEADS * DN_DK
DN_VW = DN_HEADS * DN_DV
DN_QKV_W = 2 * DN_KW + DN_VW
DN_PROJ_W = DN_QKV_W + DN_VW + 4 * DN_HEADS
CONV_K = 5
DN_CHUNK = 64
N_GROUPS = 4
EXPERTS_PER_GROUP = 8
N_EXPERTS = N_GROUPS * EXPERTS_PER_GROUP
TOP_K = 2
EXPERT_FF = 512
MOE_BLOCK = 512
N_ATTN_LAYERS = (DEPTH + 1) // 2
N_DN_LAYERS = DEPTH // 2
F32 = jnp.float32

kernel_name = 'hybrid_swa_deltanet_hmoe_dit'


def rms_norm(x, gain):
    xf = x.astype(F32)
    y = xf * lax.rsqrt(jnp.mean(xf * xf, axis=-1, keepdims=True) + EPS) * gain.astype(F32)
    return y.astype(x.dtype)


def l2_normalize(x):
    return x * lax.rsqrt(jnp.sum(x * x, axis=-1, keepdims=True) + EPS)


def axial_rope_tables(n):
    rows = n // GRID_W
    row = jnp.repeat(jnp.arange(rows, dtype=F32), GRID_W)
    col = jnp.tile(jnp.arange(GRID_W, dtype=F32), rows)
    inv = ROPE_BASE ** (-jnp.arange(ROPE_FREQS, dtype=F32) / ROPE_FREQS)
    ang = jnp.stack([row[:, None] * inv, col[:, None] * inv], axis=1)
    return jnp.cos(ang), jnp.sin(ang)


def apply_axial_rope(x, cos, sin):
    b, t, hh, dh = x.shape
    xr = x.astype(F32).reshape(b, t, hh, 2, 2, ROPE_FREQS)
    x0, x1 = xr[..., 0, :], xr[..., 1, :]
    cs, sn = cos[None, :, None], sin[None, :, None]
    out = jnp.stack([x0 * cs - x1 * sn, x1 * cs + x0 * sn], axis=-2)
    return out.reshape(b, t, hh, dh).astype(x.dtype)


def head_rms(x, gain):
    xf = x.astype(F32)
    return (xf * lax.rsqrt(jnp.mean(xf * xf, axis=-1, keepdims=True) + EPS) * gain.astype(F32)).astype(x.dtype)


def window_attention(q, k, v, kc, vc, sink):
    b, n, h, dh = q.shape
    g = k.shape[2]
    r = h // g
    nb = n // ATT_BLOCK
    scale = dh ** -0.5
    qb = q.reshape(b, nb, ATT_BLOCK, g, r, dh).transpose(1, 0, 2, 3, 4, 5)

    def band(t):
        tp = jnp.pad(t, ((0, 0), (ATT_BLOCK, ATT_BLOCK), (0, 0), (0, 0))).reshape(b, nb + 2, ATT_BLOCK, g, dh)
        w = jnp.concatenate([tp[:, :-2], tp[:, 1:-1], tp[:, 2:]], axis=2)
        return w.transpose(1, 0, 2, 3, 4)

    kw, vw = band(k), band(v)
    qi = jnp.arange(ATT_BLOCK)
    kj = jnp.arange(3 * ATT_BLOCK)
    rel = kj[None, :] - ATT_BLOCK - qi[:, None]
    kabs = (jnp.arange(nb)[:, None] - 1) * ATT_BLOCK + kj[None, :]
    mask = (jnp.abs(rel) <= WINDOW)[None] & ((kabs >= 0) & (kabs < n))[:, None, :]
    sk = sink.astype(F32).reshape(1, g, r, 1, 1)
    kc32, vc32 = kc.astype(F32), vc.astype(F32)

    def one_block(args):
        qblk, kblk, vblk, mblk = args
        s_w = jnp.einsum('bqgrd,bkgd->bgrqk', qblk, kblk, preferred_element_type=F32) * scale
        s_w = jnp.where(mblk, s_w, -jnp.inf)
        s_c = jnp.einsum('bqgrd,bkgd->bgrqk', qblk.astype(F32), kc32) * scale
        m = jnp.maximum(jnp.maximum(s_w.max(-1, keepdims=True), s_c.max(-1, keepdims=True)), sk)
        p_w = jnp.exp(s_w - m)
        p_c = jnp.exp(s_c - m)
        den = p_w.sum(-1) + p_c.sum(-1) + jnp.exp(sk - m)[..., 0]
        o = jnp.einsum('bgrqk,bkgd->bqgrd', p_w, vblk.astype(F32)) + jnp.einsum('bgrqk,bkgd->bqgrd', p_c, vc32)
        return (o / den.transpose(0, 3, 1, 2)[..., None]).astype(qblk.dtype)

    o = lax.map(one_block, (qb, kw, vw, mask))
    return o.transpose(1, 0, 2, 3, 4, 5).reshape(b, n, h * dh)


def context_attention(qc, kc, vc, sink):
    b, l, h, dh = qc.shape
    g = kc.shape[2]
    r = h // g
    qg = qc.reshape(b, l, g, r, dh)
    s = jnp.einsum('bqgrd,bkgd->bgrqk', qg, kc, preferred_element_type=F32) * dh ** -0.5
    sk = sink.astype(F32).reshape(1, g, r, 1, 1)
    m = jnp.maximum(s.max(-1, keepdims=True), sk)
    p = jnp.exp(s - m)
    den = p.sum(-1) + jnp.exp(sk - m)[..., 0]
    o = jnp.einsum('bgrqk,bkgd->bqgrd', p, vc.astype(F32)) / den.transpose(0, 3, 1, 2)[..., None]
    return o.reshape(b, l, h * dh).astype(qc.dtype)


def attention_mixer(h_lat, h_ctx, w_qkv, w_o, q_gain, k_gain, sink, cos, sin, ctx_out):
    def project(h):
        bsz, t_len, _ = h.shape
        p = h @ w_qkv
        q = p[..., :ATT_Q_W].reshape(bsz, t_len, ATT_HEADS, HEAD_DIM)
        k = p[..., ATT_Q_W:ATT_Q_W + ATT_KV_W].reshape(bsz, t_len, ATT_KV_HEADS, HEAD_DIM)
        v = p[..., ATT_Q_W + ATT_KV_W:].reshape(bsz, t_len, ATT_KV_HEADS, HEAD_DIM)
        return head_rms(q, q_gain), head_rms(k, k_gain), v

    ql, kl, vl = project(h_lat)
    ql, kl = apply_axial_rope(ql, cos, sin), apply_axial_rope(kl, cos, sin)
    qc, kc, vc = project(h_ctx)
    y_lat = window_attention(ql, kl, vl, kc, vc, sink) @ w_o
    y_ctx = context_attention(qc, kc, vc, sink) @ w_o if ctx_out else None
    return y_lat, y_ctx


def centred_depthwise_conv(x, w):
    ch = x.shape[-1]
    return lax.conv_general_dilated(x, w[:, None, :].astype(x.dtype), window_strides=(1,),
                                    padding=[(CONV_K // 2, CONV_K // 2)],
                                    dimension_numbers=('NWC', 'WIO', 'NWC'), feature_group_count=ch)


def gated_delta_chunked(q, k, v, g, beta, s0, with_out):
    b, t_len, h, dk = k.shape
    dv = v.shape[-1]
    nc = t_len // DN_CHUNK

    def chunks(t):
        return jnp.moveaxis(t.reshape(b, nc, DN_CHUNK, h, *t.shape[3:]), 3, 1)

    q = chunks(q) * dk ** -0.5
    k, v, g, beta = chunks(k), chunks(v), chunks(g), chunks(beta)
    gcum = jnp.cumsum(g, axis=-1)
    tril = jnp.tril(jnp.ones((DN_CHUNK, DN_CHUNK), bool))
    stril = jnp.tril(jnp.ones((DN_CHUNK, DN_CHUNK), bool), -1)
    decay = jnp.exp(jnp.where(tril, gcum[..., :, None] - gcum[..., None, :], -jnp.inf))
    kb = k * beta[..., None]
    lmat = jnp.where(stril, jnp.einsum('bhncd,bhnsd->bhncs', kb, k) * decay, 0.0)
    a_mat = lmat + jnp.eye(DN_CHUNK, dtype=F32)
    rhs = jnp.concatenate([v * beta[..., None], kb * jnp.exp(gcum)[..., None]], axis=-1)
    sol = lax.linalg.triangular_solve(a_mat, rhs, left_side=True, lower=True, unit_diagonal=True)
    u, w = sol[..., :dv], sol[..., dv:]
    glast = gcum[..., -1:]
    kd = k * jnp.exp(glast - gcum)[..., None]
    eg = jnp.exp(glast)[..., None]
    if with_out:
        qg = q * jnp.exp(gcum)[..., None]
        qk = jnp.where(tril, jnp.einsum('bhncd,bhnsd->bhncs', q, k) * decay, 0.0)
        xs = tuple(jnp.moveaxis(t, 2, 0) for t in (u, w, kd, eg, qg, qk))

        def step(s, xs_i):
            u_i, w_i, kd_i, eg_i, qg_i, qk_i = xs_i
            v_new = u_i - jnp.einsum('bhck,bhkv->bhcv', w_i, s)
            o = jnp.einsum('bhck,bhkv->bhcv', qg_i, s) + jnp.einsum('bhcs,bhsv->bhcv', qk_i, v_new)
            return s * eg_i + jnp.einsum('bhck,bhcv->bhkv', kd_i, v_new), o

        s_fin, o = lax.scan(step, s0, xs)
        return o.transpose(1, 0, 3, 2, 4).reshape(b, t_len, h, dv), s_fin
    xs = tuple(jnp.moveaxis(t, 2, 0) for t in (u, w, kd, eg))

    def state_step(s, xs_i):
        u_i, w_i, kd_i, eg_i = xs_i
        v_new = u_i - jnp.einsum('bhck,bhkv->bhcv', w_i, s)
        return s * eg_i + jnp.einsum('bhck,bhcv->bhkv', kd_i, v_new), None

    s_fin, _ = lax.scan(state_step, s0, xs)
    return None, s_fin


def gated_out(o, z, o_gain, w_o):
    b, t_len = o.shape[:2]
    zf = z.astype(F32).reshape(b, t_len, DN_HEADS, DN_DV)
    y = o * lax.rsqrt(jnp.mean(o * o, axis=-1, keepdims=True) + EPS) * o_gain.astype(F32) * jax.nn.silu(zf)
    return y.reshape(b, t_len, DN_VW).astype(z.dtype) @ w_o


def deltanet_mixer(h_lat, h_ctx, w_in, conv_w, a_log, dt_bias, o_gain, w_o, ctx_out):
    def project(h):
        bsz, t_len, _ = h.shape
        p = h @ w_in
        qkv = jax.nn.silu(centred_depthwise_conv(p[..., :DN_QKV_W], conv_w)).astype(F32)
        z = p[..., DN_QKV_W:DN_QKV_W + DN_VW]
        ab = p[..., DN_QKV_W + DN_VW:].astype(F32).reshape(bsz, t_len, 2, 2, DN_HEADS)
        q = l2_normalize(qkv[..., :DN_KW].reshape(bsz, t_len, DN_HEADS, DN_DK))
        k = l2_normalize(qkv[..., DN_KW:2 * DN_KW].reshape(bsz, t_len, DN_HEADS, DN_DK))
        v = qkv[..., 2 * DN_KW:].reshape(bsz, t_len, DN_HEADS, DN_DV)
        g = -jnp.exp(a_log.astype(F32)) * jax.nn.softplus(ab[:, :, 0] + dt_bias.astype(F32))
        beta = jax.nn.sigmoid(ab[:, :, 1])
        return q, k, v, g, beta, z

    qc, kc, vc, gc, bc, zc = project(h_ctx)
    ql, kl, vl, gl, bl, zl = project(h_lat)
    s0 = jnp.zeros((h_lat.shape[0], DN_HEADS, DN_DK, DN_DV), F32)
    rev = lambda t: jnp.flip(t, axis=1)
    oc_f, sc_f = gated_delta_chunked(qc, kc, vc, gc[:, :, 0], bc[:, :, 0], s0, ctx_out)
    ol_f, _ = gated_delta_chunked(ql, kl, vl, gl[:, :, 0], bl[:, :, 0], sc_f, True)
    oc_b, sc_b = gated_delta_chunked(rev(qc), rev(kc), rev(vc), rev(gc[:, :, 1]), rev(bc[:, :, 1]), s0, ctx_out)
    ol_b, _ = gated_delta_chunked(rev(ql), rev(kl), rev(vl), rev(gl[:, :, 1]), rev(bl[:, :, 1]), sc_b, True)
    y_lat = gated_out(ol_f + rev(ol_b), zl, o_gain, w_o)
    y_ctx = gated_out(oc_f + rev(oc_b), zc, o_gain, w_o) if ctx_out else None
    return y_lat, y_ctx


def hier_moe(h, w_group, b_group, w_expert, b_expert, w_in, w_out):
    n_tok, d = h.shape
    hf = h.astype(F32)
    p_group = jax.nn.softmax(hf @ w_group.astype(F32) + b_group.astype(F32), axis=-1)
    p_top, g_idx = lax.top_k(p_group, 1)
    le = (hf @ w_expert.astype(F32) + b_expert.astype(F32)).reshape(n_tok, N_GROUPS, EXPERTS_PER_GROUP)
    le_g = jnp.take_along_axis(le, g_idx[:, :, None], axis=1)[:, 0]
    e_val, e_idx = lax.top_k(le_g, TOP_K)
    gate = (jax.nn.softmax(e_val, axis=-1) * p_top).reshape(-1)
    eid = (g_idx * EXPERTS_PER_GROUP + e_idx).reshape(-1)
    n_slot = n_tok * TOP_K
    tok = jnp.arange(n_slot, dtype=jnp.int32) // TOP_K
    order = jnp.argsort(eid)
    e_sorted = eid[order]
    counts = jnp.bincount(eid, length=N_EXPERTS)
    padded = (counts + MOE_BLOCK - 1) // MOE_BLOCK * MOE_BLOCK
    ends_pad = jnp.cumsum(padded)
    start_pad = ends_pad - padded
    start = jnp.cumsum(counts) - counts
    dest = start_pad[e_sorted] + jnp.arange(n_slot, dtype=jnp.int32) - start[e_sorted]
    n_blocks = (n_slot + N_EXPERTS * (MOE_BLOCK - 1)) // MOE_BLOCK
    n_buf = n_blocks * MOE_BLOCK
    buf_tok = jnp.full((n_buf,), n_tok, jnp.int32).at[dest].set(tok[order])
    buf_gate = jnp.zeros((n_buf,), F32).at[dest].set(gate[order])
    blk_e = jnp.minimum(jnp.searchsorted(ends_pad, jnp.arange(n_blocks) * MOE_BLOCK, side='right'), N_EXPERTS - 1)
    h_pad = jnp.concatenate([h, jnp.zeros((1, d), h.dtype)], axis=0)
    xb = h_pad[buf_tok].reshape(n_blocks, MOE_BLOCK, d)

    def expert_block(args):
        xblk, e = args
        hu = xblk @ w_in[e]
        return (jax.nn.silu(hu[:, :EXPERT_FF]) * hu[:, EXPERT_FF:]) @ w_out[e]

    yb = lax.map(expert_block, (xb, blk_e)).reshape(n_buf, d)
    y = jnp.zeros((n_tok + 1, d), F32).at[buf_tok].add(yb.astype(F32) * buf_gate[:, None])
    return y[:n_tok].astype(h.dtype)


def setup_inputs(seed: int = 0) -> dict:
    key = jax.random.key(seed)
    ks = jax.random.split(key, 24)
    D = D_MODEL

    def nrm(k, shape, s):
        return jax.random.normal(k, shape, F32) * s

    dt = jnp.exp(jax.random.uniform(ks[15], (N_DN_LAYERS, 2, DN_HEADS), F32, math.log(1e-3), math.log(1e-1)))
    return {
        'x': nrm(ks[0], (BATCH, SEQ, D), 1.0),
        'c': nrm(ks[1], (BATCH, D), 1.0),
        'ctx': nrm(ks[2], (BATCH, CTX_LEN, D), 1.0),
        'c_ctx': nrm(ks[3], (D,), 1.0),
        'ada_w': nrm(ks[4], (DEPTH, D, N_MOD * D), 0.5 * D ** -0.5),
        'ada_b': nrm(ks[5], (DEPTH, N_MOD * D), 0.02),
        'norm_g': 1.0 + nrm(ks[6], (DEPTH, 2, D), 0.02),
        'attn_w_qkv': nrm(ks[7], (N_ATTN_LAYERS, D, ATT_PROJ_W), D ** -0.5),
        'attn_w_o': nrm(ks[8], (N_ATTN_LAYERS, ATT_Q_W, D), ATT_Q_W ** -0.5),
        'attn_q_gain': 1.0 + nrm(ks[9], (N_ATTN_LAYERS, HEAD_DIM), 0.02),
        'attn_k_gain': 1.0 + nrm(ks[10], (N_ATTN_LAYERS, HEAD_DIM), 0.02),
        'attn_sink': nrm(ks[11], (N_ATTN_LAYERS, ATT_HEADS), 0.5),
        'dn_w_in': nrm(ks[12], (N_DN_LAYERS, D, DN_PROJ_W), D ** -0.5),
        'dn_conv_w': nrm(ks[13], (N_DN_LAYERS, CONV_K, DN_QKV_W), CONV_K ** -0.5),
        'dn_a_log': jnp.log(jax.random.uniform(ks[14], (N_DN_LAYERS, 2, DN_HEADS), F32, 1.0, 16.0)),
        'dn_dt_bias': dt + jnp.log(-jnp.expm1(-dt)),
        'dn_o_gain': 1.0 + nrm(ks[16], (N_DN_LAYERS, DN_DV), 0.02),
        'dn_w_o': nrm(ks[17], (N_DN_LAYERS, DN_VW, D), DN_VW ** -0.5),
        'moe_w_group': nrm(ks[18], (DEPTH, D, N_GROUPS), D ** -0.5),
        'moe_b_group': nrm(ks[19], (DEPTH, N_GROUPS), 0.01),
        'moe_w_expert': nrm(ks[20], (DEPTH, D, N_EXPERTS), D ** -0.5),
        'moe_b_expert': nrm(ks[21], (DEPTH, N_EXPERTS), 0.01),
        'moe_w_in': nrm(ks[22], (DEPTH, N_EXPERTS, D, 2 * EXPERT_FF), D ** -0.5),
        'moe_w_out': nrm(ks[23], (DEPTH, N_EXPERTS, EXPERT_FF, D), EXPERT_FF ** -0.5),
    }


def reference(x, c, ctx, c_ctx, ada_w, ada_b, norm_g, attn_w_qkv, attn_w_o, attn_q_gain, attn_k_gain,
              attn_sink, dn_w_in, dn_conv_w, dn_a_log, dn_dt_bias, dn_o_gain, dn_w_o, moe_w_group,
              moe_b_group, moe_w_expert, moe_b_expert, moe_w_in, moe_w_out):
    b, n, d = x.shape
    l = ctx.shape[1]
    cos, sin = axial_rope_tables(n)
    s_lat = jax.nn.silu(c.astype(F32))
    s_ctx = jax.nn.silu(c_ctx.astype(F32))
    h_lat, h_ctx = x, ctx
    for i in range(DEPTH):
        last = i == DEPTH - 1
        j = i // N_MIXERS
        mod_l = (s_lat @ ada_w[i].astype(F32) + ada_b[i].astype(F32)).reshape(b, 1, N_MOD, d).astype(x.dtype)
        mod_c = (s_ctx @ ada_w[i].astype(F32) + ada_b[i].astype(F32)).reshape(N_MOD, d).astype(x.dtype)
        sh1_l, sc1_l, g1_l, sh2_l, sc2_l, g2_l = [mod_l[:, :, m] for m in range(N_MOD)]
        sh1_c, sc1_c, g1_c, sh2_c, sc2_c, g2_c = [mod_c[m] for m in range(N_MOD)]
        a_l = rms_norm(h_lat, norm_g[i, 0]) * (1 + sc1_l) + sh1_l
        a_c = rms_norm(h_ctx, norm_g[i, 0]) * (1 + sc1_c) + sh1_c
        if i % N_MIXERS == 0:
            y_l, y_c = attention_mixer(a_l, a_c, attn_w_qkv[j], attn_w_o[j], attn_q_gain[j], attn_k_gain[j],
                                       attn_sink[j], cos, sin, not last)
        else:
            y_l, y_c = deltanet_mixer(a_l, a_c, dn_w_in[j], dn_conv_w[j], dn_a_log[j], dn_dt_bias[j],
                                      dn_o_gain[j], dn_w_o[j], not last)
        h_lat = h_lat + g1_l * y_l
        m_l = rms_norm(h_lat, norm_g[i, 1]) * (1 + sc2_l) + sh2_l
        if last:
            y = hier_moe(m_l.reshape(-1, d), moe_w_group[i], moe_b_group[i], moe_w_expert[i], moe_b_expert[i],
                         moe_w_in[i], moe_w_out[i])
            h_lat = h_lat + g2_l * y.reshape(b, n, d)
        else:
            h_ctx = h_ctx + g1_c * y_c
            m_c = rms_norm(h_ctx, norm_g[i, 1]) * (1 + sc2_c) + sh2_c
            toks = jnp.concatenate([m_l.reshape(-1, d), m_c.reshape(-1, d)], axis=0)
            y = hier_moe(toks, moe_w_group[i], moe_b_group[i], moe_w_expert[i], moe_b_expert[i],
                         moe_w_in[i], moe_w_out[i])
            h_lat = h_lat + g2_l * y[:b * n].reshape(b, n, d)
            h_ctx = h_ctx + g2_c * y[b * n:].reshape(b, l, d)
    return h_lat
```

```python
import numpy as np
from contextlib import ExitStack
import concourse.bass as bass
import concourse.mybir as mybir
from concourse.bass_utils import run_bass_kernel_spmd

F32 = mybir.dt.float32
BF16 = mybir.dt.bfloat16
I32 = mybir.dt.int32
AF = mybir.ActivationFunctionType
ALU = mybir.AluOpType
AX = mybir.AxisListType

COMPUTE = ("pe", "act", "dve", "pool")
EPOCH = 12000

D = 1024
NOWN = 4096
NHALO = 256
NLAT = NOWN + NHALO
NT_LAT = NLAT // 128
NQ_LAT = 33
LCTX = 256
EPS = 1e-6
NEG = -30000.0


class Sched:
    def __init__(self, nc, stack, dma_pool=None):
        self.nc = nc
        self.stack = stack
        self.ops = []
        self.dma_pool_sizes = dma_pool or {"sp": 24, "act": 8, "pool": 16}

    def op(self, eng, fn, reads=(), writes=(), dma=False):
        if eng in ("act", "dve") and not dma:
            extra = [k for k in reads if isinstance(k, str) and len(k) == 2 and k[0] == "B" and k[1].isdigit() and k not in writes]
            writes = tuple(writes) + tuple(extra)
        self.ops.append(dict(eng=eng, fn=fn, reads=("ALL",) + tuple(reads), writes=tuple(writes), dma=dma))

    def pe(self, fn, reads=(), writes=()):
        self.op("pe", fn, reads, writes)

    def act(self, fn, reads=(), writes=()):
        self.op("act", fn, reads, writes)

    def dve(self, fn, reads=(), writes=()):
        self.op("dve", fn, reads, writes)

    def pool(self, fn, reads=(), writes=()):
        self.op("pool", fn, reads, writes)

    def dma(self, q, out, in_, reads=(), writes=(), **kw):
        self.op(q, lambda e: e.dma_start(out=out, in_=in_, **kw), reads, writes, dma=True)

    def barrier(self, scratch):
        self.ops.append(dict(eng="pool", fn=lambda e: e.memset(scratch, 0.0), reads=(), writes=("ALL",), dma=False))

    def emit(self, final_keys=()):
        nc = self.nc
        ops = self.ops
        ops.append(dict(eng="sp", fn=None, reads=tuple(final_keys), writes=(), dma=False))
        n = len(ops)
        writers, readers = {}, {}
        deps = [None] * n

        def actor(i):
            o = ops[i]
            return ("dma", i) if o["dma"] else o["eng"]

        for i, o in enumerate(ops):
            d = set()
            a = actor(i)
            for k in o["reads"]:
                d.update(writers.get(k, {}).values())
            for k in o["writes"]:
                d.update(writers.get(k, {}).values())
                d.update(readers.get(k, {}).values())
            for k in o["writes"]:
                writers[k] = {a: i}
                readers[k] = {}
            for k in o["reads"]:
                if k in o["writes"]:
                    continue
                readers.setdefault(k, {})[a] = i
            d.discard(i)
            if o["eng"] == "pe" and not o["dma"]:
                d = {j for j in d if not (ops[j]["eng"] == "pe" and not ops[j]["dma"])}
            deps[i] = d
        needed = set()
        for d in deps:
            needed |= d
        eng_sems = {}
        eng_count = {e: 0 for e in COMPUTE}

        def new_sem(name):
            return self.stack.enter_context(nc.semaphore(name))

        for e in COMPUTE:
            eng_sems[e] = [new_sem(f"s_{e}_0")]
        dma_sems = {q: [new_sem(f"d_{q}_{i}") for i in range(sz)] for q, sz in self.dma_pool_sizes.items()}
        dma_rr = {q: 0 for q in dma_sems}
        dma_val = {q: [0] * len(dma_sems[q]) for q in dma_sems}
        dma_last = {q: [None] * len(dma_sems[q]) for q in dma_sems}
        signal = [None] * n
        extra_wait = [None] * n
        for i, o in enumerate(ops):
            if o["dma"]:
                q = o["eng"]
                s = dma_rr[q]
                dma_rr[q] = (s + 1) % len(dma_sems[q])
                if dma_last[q][s] is not None:
                    extra_wait[i] = (dma_sems[q][s], dma_val[q][s])
                dma_val[q][s] += 16
                dma_last[q][s] = i
                signal[i] = (dma_sems[q][s], dma_val[q][s], 16)
            elif i in needed and o["eng"] in COMPUTE:
                e = o["eng"]
                if eng_count[e] >= EPOCH:
                    eng_sems[e].append(new_sem(f"s_{e}_{len(eng_sems[e])}"))
                    eng_count[e] = 0
                eng_count[e] += 1
                signal[i] = (eng_sems[e][-1], eng_count[e], 1)
        streams = {}
        for i, o in enumerate(ops):
            streams.setdefault(o["eng"], []).append(i)
        self.n_waits = 0

        def run_engine(eng_name, e):
            seen = {}
            for i in streams.get(eng_name, []):
                o = ops[i]
                waits = {}
                cands = [signal[j] for j in deps[i]]
                if extra_wait[i] is not None:
                    cands.append(extra_wait[i])
                for sg in cands:
                    assert sg is not None
                    key = id(sg[0])
                    if key not in waits or waits[key][1] < sg[1]:
                        waits[key] = (sg[0], sg[1])
                for key, (sem, val) in waits.items():
                    if seen.get(key, 0) >= val:
                        continue
                    e.wait_ge(sem, val)
                    self.n_waits += 1
                    seen[key] = val
                if o["fn"] is None:
                    continue
                ins = o["fn"](e)
                if signal[i] is not None:
                    assert ins is not None, f"op {i} on {eng_name} returned None"
                    ins.then_inc(signal[i][0], signal[i][2])

        with nc.Block() as block:
            @block.sync
            def _(e):
                run_engine("sp", e)

            @block.tensor
            def _(e):
                run_engine("pe", e)

            @block.scalar
            def _(e):
                run_engine("act", e)

            @block.vector
            def _(e):
                run_engine("dve", e)

            @block.gpsimd
            def _(e):
                run_engine("pool", e)
        return n


class K:
    pass


def bc_free(ap, shape):
    return ap.rearrange("p (h o) -> p h o", o=1).to_broadcast(list(shape))


def bc_mid(ap, shape):
    return ap.rearrange("p (o d) -> p o d", o=1).to_broadcast(list(shape))


def build(debug=None, n_lat_tiles=NT_LAT, stop_after=None, mode="full"):
    debug = list(debug or [])
    nc = bass.Bass("TRN2", target_bir_lowering=False)
    k = K()
    k.nc = nc
    din = lambda name, shape, dt=F32: nc.dram_tensor(name, list(shape), dt, kind="ExternalInput").ap()

    def dscr(name, shape, dt=F32):
        kind = "ExternalOutput" if name in debug else "Internal"
        return nc.dram_tensor(name, list(shape), dt, kind=kind).ap()

    if mode == "scan_test":
        return build_scan_test(nc, k, din, dscr, debug)
    l1_outs = ["h2", "zs", "gb", "fm", "oA", "sout"]
    if mode == "L1":
        debug += l1_outs
    if mode == "T1":
        debug += l1_outs[1:]
    do_l0 = mode in ("full", "L1")
    do_dn = stop_after is None
    do_pre = do_dn and mode in ("full", "L1", "dn_test", "T1")
    do_scanA = do_pre
    do_exch = do_dn and mode in ("full", "dn_test")
    do_b = do_dn and mode in ("full", "L2", "dn_test", "T2")
    do_moe1 = do_dn and mode in ("full", "L2")
    k.lmap = {0: 0, 1: (1 if mode == "full" else 0)}
    nl = 2 if mode == "full" else 1
    k.cvec = din("cvec", [128, 16])
    k.consts = din("consts", [128, 128 * 2 + 512 * 2])
    k.ada_w = din("ada_w", [2, D, 6 * D])
    k.ada_b = din("ada_b", [2, 6 * D])
    k.norm_g = din("norm_g", [2, 2 * D])
    if do_l0:
        k.x = din("x", [NLAT, D])
        k.ctx = din("ctx", [LCTX, D])
        k.cosT = din("cosT", [NLAT, 64])
        k.sinT = din("sinT", [NLAT, 64])
        k.w_qkv = din("attn_w_qkv", [D, 1536])
        k.w_o = din("attn_w_o", [D, D])
        k.qk_gain = din("qk_gain", [128])
        k.sink = din("attn_sink", [16])
    if do_l0 or do_moe1:
        k.mconst = din("mconst", [128, 96])
        k.w_router = din("w_router", [2, D, 36])
        k.b_router = din("b_router", [2, 36])
        k.moe_w_in = din("moe_w_in", [nl, 32, D, D])
        k.moe_w_out = din("moe_w_out", [nl, 32, 512, D])
    if do_pre:
        k.dn_w_in = din("dn_w_in", [D, 4128])
        k.dn_conv = din("dn_conv", [128, 120])
        k.dn_alog = din("dn_alog", [16])
        k.dn_dtb = din("dn_dtb", [16])
    if do_pre or do_b:
        k.dnconst = din("dnconst", [64, 2 * 2176])
    if do_b:
        k.dn_ogain = din("dn_ogain", [128])
        k.dn_w_o = din("dn_w_o", [D, D])
    if do_exch:
        k.selv = din("selv", [128, 2])
    if do_l0:
        k.h1 = dscr("h1", [NQ_LAT * 128, D])
        k.h1c = dscr("h1c", [LCTX, D])
        k.h2 = dscr("h2", [NQ_LAT * 128, D])
        k.h2c = dscr("h2c", [LCTX, D])
    else:
        k.h2 = din("h2", [NQ_LAT * 128, D])
        if do_pre:
            k.h2c = din("h2c", [LCTX, D])
    k.dbg = dscr("dbg", [128, 256]) if "dbg" in debug else None
    if do_l0 or do_moe1:
        NBmax = moe_nblocks(NQ_LAT + 2)
        k.mbuf = dscr("mbuf", [(NQ_LAT + 2) * 128, D], BF16)
        k.xs = dscr("xs", [NBmax * MB, D], BF16)
        k.ys = dscr("ys", [NBmax * MB, D])
    if do_dn:
        mk = dscr if do_pre else din
        k.zs = mk("zs", [NOWN, D], BF16)
        k.gb = mk("gb", [NTOKP, 32])
        k.fm = mk("fm", [24, 128, NTOKP], BF16)
        k.oA = mk("oA", [NOWN, D])
        if do_scanA:
            k.sout = dscr("sout", [1024, 128])
        if do_exch:
            k.sgat = dscr("sgat", [2048, 128])
        if do_b:
            k.oB = dscr("oB", [NOWN, D])
            k.sin = dscr("sin", [1024, 128]) if do_exch else din("sin", [1024, 128])
            k.h3 = dscr("h3", [NOWN, D])
    out_rows = NOWN if mode in ("full", "L2") else 256
    k.out = nc.dram_tensor("out", [out_rows, D], F32, kind="ExternalOutput").ap()

    with ExitStack() as st:
        S = Sched(nc, st)
        k.S = S
        k.banks = [st.enter_context(nc.psum_tensor(f"B{i}", [128, 512], F32)) for i in range(8)]
        k.bbf = [b[:].bitcast(BF16) for b in k.banks]
        k.bar = st.enter_context(nc.sbuf_tensor("barscr", [128, 1], F32))
        k.eps = st.enter_context(nc.sbuf_tensor("epsT", [128, 1], F32))
        S.pool(lambda e: e.memset(k.eps[:], EPS), writes=["eps"])
        if do_l0:
            with ExitStack() as st0:
                k.st = st0
                phase_mod(k, 0)
                with ExitStack() as sta:
                    k.st = sta
                    phase_attn(k, n_lat_tiles)
                    S.barrier(k.bar[:])
                k.st = st0
                nq = min(NQ_LAT, n_lat_tiles - 1)
                if stop_after != "attn":
                    toks = [(k.h1[i * 128:(i + 1) * 128, :], "h1", k.modl, k.h2[i * 128:(i + 1) * 128, :], "h2") for i in range(nq)]
                    toks += [(k.h1c[c * 128:(c + 1) * 128, :], "h1c", k.modc, k.h2c[c * 128:(c + 1) * 128, :], "h2c") for c in range(2)]
                    phase_moe(k, 0, toks)
                S.barrier(k.bar[:])
        if do_dn:
            with ExitStack() as st1:
                k.st = st1
                phase_mod(k, 1)
                if do_pre:
                    phase_dn_pre(k)
                    phase_dn_scan(k, "A")
                if do_exch:
                    phase_dn_exchange(k)
                if do_b:
                    phase_dn_scan(k, "B")
                    phase_dn_out(k)
                if do_moe1:
                    toks = [(k.h3[i * 128:(i + 1) * 128, :], "h3", k.modl, k.out[i * 128:(i + 1) * 128, :], "out") for i in range(32)]
                    phase_moe(k, 1, toks)
                S.barrier(k.bar[:])
        final = ["out"] + [d for d in debug]
        if not do_moe1:
            with ExitStack() as st1:
                t = st1.enter_context(nc.sbuf_tensor("cp", [128, D], F32))
                S.pool(lambda e: e.memset(t[:], 0.0), writes=["cp"])
                for n in range(2):
                    S.dma("sp", k.out[n * 128:(n + 1) * 128, :], t[:], reads=["cp"], writes=["out"])
        nops = S.emit(final_keys=final)
        print("ops", nops, "waits", S.n_waits)
    return nc


def build_scan_test(nc, k, din, dscr, debug):
    k.consts = din("consts", [128, 128 * 2 + 512 * 2])
    k.dnconst = din("dnconst", [64, 2 * 2176])
    k.fm = din("fm", [24, 128, NTOKP], BF16)
    k.gb = din("gb", [NTOKP, 32])
    k.oA = dscr("oA", [NOWN, D])
    k.oB = dscr("oB", [NOWN, D])
    k.sout = dscr("sout", [1024, 128])
    if "SCANB" in debug:
        k.sin = din("sin", [1024, 128])
    k.out = nc.dram_tensor("out", [128, D], F32, kind="ExternalOutput").ap()
    with ExitStack() as st:
        S = Sched(nc, st)
        k.S = S
        k.banks = [st.enter_context(nc.psum_tensor(f"B{i}", [128, 512], F32)) for i in range(8)]
        k.bbf = [b[:].bitcast(BF16) for b in k.banks]
        k.bar = st.enter_context(nc.sbuf_tensor("barscr", [128, 1], F32))
        k.eps = st.enter_context(nc.sbuf_tensor("epsT", [128, 1], F32))
        S.pool(lambda e: e.memset(k.eps[:], EPS), writes=["eps"])
        phase_dn_scan(k, "B" if "SCANB" in debug else "A")
        t = st.enter_context(nc.sbuf_tensor("cp", [128, D], F32))
        S.pool(lambda e: e.memset(t[:], 0.0), writes=["cp"])
        S.dma("sp", k.out, t[:], reads=["cp"], writes=["out"])
        nops = S.emit(final_keys=["out"] + [d for d in debug if d in ("oA", "oB", "sout")])
        print("ops", nops, "waits", S.n_waits)
    return nc


def stop_after_dn(debug):
    if "DN1" in debug:
        return 1
    if "DN2" in debug:
        return 2
    return 3


def T(k, name, shape, dt):
    return k.st.enter_context(k.nc.sbuf_tensor(name, list(shape), dt))


def phase_mod(k, layer):
    S, nc = k.S, k.nc
    L = layer
    modl = T(k, f"modl{L}", [128, 6 * D], F32)
    modc = T(k, f"modc{L}", [128, 6 * D], F32)
    k.modl, k.modc = modl, modc
    with ExitStack() as stt:
        TT = lambda name, shape, dt: stt.enter_context(nc.sbuf_tensor(name, list(shape), dt))
        cv = TT(f"cv{L}", [128, 16], F32)
        sv = TT(f"sv{L}", [128, 16], F32)
        sbc = TT(f"sbc{L}", [128, 16, 128], F32)
        adab = TT(f"adab{L}", [128, 6 * D], F32)
        ng = TT(f"ng{L}", [128, 2 * D], F32)
        wt = [TT(f"adaw{L}_{i}", [128, 8, 512], F32) for i in range(2)]
        S.dma("sp", cv[:], k.cvec, writes=["cv"])
        S.dma("sp", adab[:], k.ada_b[L].partition_broadcast(128), writes=["adab"])
        S.dma("sp", ng[:], k.norm_g[L].partition_broadcast(128), writes=["ng"])
        S.act(lambda e: e.activation(out=sv[:], in_=cv[:], func=AF.Silu), reads=["cv"], writes=["sv"])
        S.dve(lambda e: e.tensor_copy(out=sbc[:], in_=bc_free(sv[:], [128, 16, 128])), reads=["sv"], writes=["sbc"])
        wv = k.ada_w[L].rearrange("(k p) n -> p k n", p=128)
        for n in range(12):
            w = wt[n % 2]
            wk = f"adaw{n % 2}"
            S.dma("sp" if n % 2 == 0 else "act", w[:], wv[:, :, n * 512:(n + 1) * 512], writes=[wk])
            for which, dst in ((0, modl), (1, modc)):
                bank = k.banks[which]
                bk = f"B{which}"
                for kk in range(8):
                    S.pe(lambda e, bank=bank, kk=kk, which=which, w=w: e.matmul(bank[:], lhsT=sbc[:, which * 8 + kk, :], rhs=w[:, kk, :], start=(kk == 0), stop=(kk == 7)),
                         reads=["sbc", wk], writes=[bk])
                S.dve(lambda e, bank=bank, dst=dst, n=n: e.tensor_tensor(out=dst[:, n * 512:(n + 1) * 512], in0=bank[:], in1=adab[:, n * 512:(n + 1) * 512], op=ALU.add),
                      reads=[bk, "adab"], writes=[f"mod{which}"])
        for which, dst in ((0, modl), (1, modc)):
            for (slot, gi) in ((1, 0), (4, 1)):
                S.dve(lambda e, dst=dst, slot=slot, gi=gi: e.scalar_tensor_tensor(out=dst[:, slot * D:(slot + 1) * D], in0=dst[:, slot * D:(slot + 1) * D], scalar=1.0,
                                                                            in1=ng[:, gi * D:(gi + 1) * D], op0=ALU.add, op1=ALU.mult),
                      reads=[f"mod{which}", "ng"], writes=[f"mod{which}"])
        S.barrier(k.bar[:])


def rstd_from_ssq(k, ssq, rstd, n_inv, rk, wk):
    S = k.S
    S.act(lambda e: e.activation(out=rstd, in_=ssq, func=AF.Ln, bias=k.eps[:ssq.shape[0], :], scale=n_inv), reads=[rk, "eps"], writes=[wk])
    S.act(lambda e: e.activation(out=rstd, in_=rstd, func=AF.Exp, scale=-0.5), reads=[wk], writes=[wk])


def phase_attn(k, n_lat_tiles):
    S, nc = k.S, k.nc
    B, BB = k.banks, k.bbf
    modl, modc = k.modl, k.modc
    NXT, NQ, NKV, NP = 3, 2, 4, 3
    idb = T(k, "idb", [128, 128], BF16)
    maskp = T(k, "maskp", [128, 512], BF16)
    maskn = T(k, "maskn", [128, 512], BF16)
    wqkv = T(k, "wqkv", [128, 8, 1536], BF16)
    wo = T(k, "wo", [64, 16, 1024], BF16)
    gsrc = T(k, "gsrc", [128, 128], F32)
    gain = T(k, "gain", [128, 1280], F32)
    sk = T(k, "sk", [1, 16], F32)
    ske = T(k, "ske", [1, 16], F32)
    sinkrow = T(k, "sinkrow", [1, 2048], BF16)
    onesk = T(k, "onesk", [128, 64], BF16)
    S.dma("pool", idb[:], k.consts[:, 0:128], writes=["idb"])
    S.dma("pool", maskp[:], k.consts[:, 256:768], writes=["maskp"])
    S.dma("pool", maskn[:], k.consts[:, 768:1280], writes=["maskn"])
    S.dma("pool", wqkv[:], k.w_qkv.rearrange("(k p) n -> p k n", p=128), writes=["wqkv"])
    S.dma("pool", wo[:], k.w_o.rearrange("(h p) n -> p h n", p=64), writes=["wo"])
    S.dma("sp", gsrc[:], k.qk_gain.partition_broadcast(128), writes=["gsrc"])
    S.dma("sp", sk[:], k.sink.rearrange("(o h) -> o h", o=1), writes=["sk"])
    g3 = gain[:].rearrange("p (h d) -> p h d", d=64)
    S.dve(lambda e: e.tensor_copy(out=g3[:, 0:16, :], in_=bc_mid(gsrc[:, 0:64], [128, 16, 64])), reads=["gsrc"], writes=["gain"])
    S.dve(lambda e: e.tensor_copy(out=g3[:, 16:20, :], in_=bc_mid(gsrc[:, 64:128], [128, 4, 64])), reads=["gsrc", "gain"], writes=["gain"])
    S.act(lambda e: e.activation(out=ske[:], in_=sk[:], func=AF.Exp), reads=["sk"], writes=["ske"])
    S.dve(lambda e: e.tensor_copy(out=sinkrow[:].rearrange("p (h q) -> p h q", q=128), in_=bc_free(ske[:], [1, 16, 128])), reads=["ske"], writes=["sinkrow"])
    S.pool(lambda e: e.memset(onesk[:], 1.0), writes=["onesk"])
    xt = [T(k, f"xt{i}", [128, D], F32) for i in range(NXT)]
    cs = [T(k, f"cs{i}", [128, 64], F32) for i in range(2)]
    sn = [T(k, f"sn{i}", [128, 64], F32) for i in range(2)]
    junk = T(k, "junk", [128, 1280], F32)
    ssq = T(k, "ssq", [128, 1], F32)
    rstd = T(k, "rstd", [128, 1], F32)
    af = T(k, "af", [128, D], F32)
    ab = T(k, "ab", [128, D], BF16)
    aT = T(k, "aT", [128, D], BF16)
    ssqh = T(k, "ssqh", [128, 20], F32)
    rstdh = T(k, "rstdh", [128, 20], F32)
    qn = T(k, "qn", [128, 1280], F32)
    t1 = T(k, "t1", [128, 1280], F32)
    t2 = T(k, "t2", [128, 1280], F32)
    qb = T(k, "qb", [128, 1280], BF16)
    qT = [T(k, f"qT{i}", [64, 2048], BF16) for i in range(NQ)]
    kT = [T(k, f"kT{i}", [64, 512], BF16) for i in range(NKV)]
    vt = [T(k, f"vt{i}", [128, 256], BF16) for i in range(NKV)]
    kTc = [T(k, f"kTc{i}", [64, 512], BF16) for i in range(2)]
    vtc = [T(k, f"vtc{i}", [128, 256], BF16) for i in range(2)]
    pT = [T(k, f"pT{i}", [128, 512], BF16) for i in range(NP)]
    rden = T(k, "rden", [64, 512], F32)
    attnT = T(k, "attnT", [64, 2048], BF16)
    ht = [T(k, f"ht{i}", [128, D], F32) for i in range(2)]

    def pre(src_rows, mod, rope_rows, sx, qslot, kT_dst, v_dst, kkey, vkey):
        xk = f"xt{sx}"
        x_ = xt[sx]
        S.dma("sp", x_[:], src_rows, writes=[xk])
        if rope_rows is not None:
            n, cslot = rope_rows
            S.dma("sp", cs[cslot][:], k.cosT[n * 128:(n + 1) * 128, :], writes=[f"cs{cslot}"])
            S.dma("sp", sn[cslot][:], k.sinT[n * 128:(n + 1) * 128, :], writes=[f"sn{cslot}"])
        S.pool(lambda e: e.memset(ssq[:], 0.0), writes=["ssq"])
        S.act(lambda e: e.activation(out=junk[:, 0:D], in_=x_[:], func=AF.Square, accum_out=ssq[:]), reads=[xk], writes=["junk", "ssq"])
        rstd_from_ssq(k, ssq[:], rstd[:], 1.0 / D, "ssq", "rstd")
        S.dve(lambda e: e.scalar_tensor_tensor(out=af[:], in0=x_[:], scalar=rstd[:, 0:1], in1=mod[:, 1 * D:2 * D], op0=ALU.mult, op1=ALU.mult),
              reads=[xk, "rstd", "mod"], writes=["af"])
        S.dve(lambda e: e.tensor_tensor(out=ab[:], in0=af[:], in1=mod[:, 0:D], op=ALU.add), reads=["af", "mod"], writes=["ab"])
        for kk in range(8):
            S.pe(lambda e, kk=kk: e.transpose(out=BB[0][:, kk * 128:(kk + 1) * 128], in_=ab[:, kk * 128:(kk + 1) * 128], identity=idb[:]),
                 reads=["ab", "idb"], writes=["B0"])
        S.act(lambda e: e.copy(out=aT[:], in_=BB[0][:, 0:D]), reads=["B0"], writes=["aT"])
        for nchunk in range(3):
            for kk in range(8):
                S.pe(lambda e, nchunk=nchunk, kk=kk: e.matmul(B[1 + nchunk][:], lhsT=aT[:, kk * 128:(kk + 1) * 128], rhs=wqkv[:, kk, nchunk * 512:(nchunk + 1) * 512],
                                                              start=(kk == 0), stop=(kk == 7)),
                     reads=["aT", "wqkv"], writes=[f"B{1 + nchunk}"])
        S.act(lambda e: e.copy(out=v_dst[:], in_=B[3][:, 256:512]), reads=["B3"], writes=[vkey])
        S.act(lambda e: e.activation(out=junk[:, 0:512], in_=B[1][:], func=AF.Square), reads=["B1"], writes=["junk"])
        S.act(lambda e: e.activation(out=junk[:, 512:1024], in_=B[2][:], func=AF.Square), reads=["B2", "junk"], writes=["junk"])
        S.act(lambda e: e.activation(out=junk[:, 1024:1280], in_=B[3][:, 0:256], func=AF.Square), reads=["B3", "junk"], writes=["junk"])
        S.dve(lambda e: e.tensor_reduce(out=ssqh[:], in_=junk[:].rearrange("p (h d) -> p h d", d=64), axis=AX.X, op=ALU.add), reads=["junk"], writes=["ssqh"])
        rstd_from_ssq(k, ssqh[:], rstdh[:], 1.0 / 64, "ssqh", "rstdh")
        for bi, (c0, c1, h0, nh) in enumerate(((0, 512, 0, 8), (512, 1024, 8, 8), (1024, 1280, 16, 4))):
            S.dve(lambda e, bi=bi, c0=c0, c1=c1, h0=h0, nh=nh: e.tensor_tensor(
                out=qn[:, c0:c1].rearrange("p (h d) -> p h d", d=64), in0=B[1 + bi][:, 0:c1 - c0].rearrange("p (h d) -> p h d", d=64),
                in1=bc_free(rstdh[:, h0:h0 + nh], [128, nh, 64]), op=ALU.mult), reads=[f"B{1 + bi}", "rstdh", "qn"], writes=["qn"])
        S.dve(lambda e: e.tensor_tensor(out=qn[:], in0=qn[:], in1=gain[:], op=ALU.mult), reads=["qn", "gain"], writes=["qn"])
        if rope_rows is not None:
            n, cslot = rope_rows
            c_, s_ = cs[cslot], sn[cslot]
            ck, sk_ = f"cs{cslot}", f"sn{cslot}"
            S.dve(lambda e: e.tensor_tensor(out=t1[:].rearrange("p (h d) -> p h d", d=64), in0=qn[:].rearrange("p (h d) -> p h d", d=64),
                                            in1=bc_mid(c_[:], [128, 20, 64]), op=ALU.mult), reads=["qn", ck], writes=["t1"])
            q5 = qn[:].rearrange("p (h a t i) -> p h a t i", h=20, a=2, t=2, i=16)
            t5 = t2[:].rearrange("p (h a t i) -> p h a t i", h=20, a=2, t=2, i=16)
            s4 = s_[:].rearrange("p (a t i) -> p a t i", a=2, t=2, i=16)
            for a in range(2):
                for t in range(2):
                    S.pool(lambda e, a=a, t=t: e.tensor_tensor(out=t5[:, :, a, t, :], in0=q5[:, :, a, 1 - t, :], in1=bc_mid(s4[:, a, t, :], [128, 20, 16]), op=ALU.mult),
                           reads=["qn", sk_, "t2"], writes=["t2"])
            S.dve(lambda e: e.tensor_tensor(out=qb[:], in0=t1[:], in1=t2[:], op=ALU.add), reads=["t1", "t2"], writes=["qb"])
        else:
            S.dve(lambda e: e.tensor_copy(out=qb[:], in_=qn[:]), reads=["qn"], writes=["qb"])
        for h in range(20):
            bi = 1 + h // 8
            S.pe(lambda e, h=h, bi=bi: e.transpose(out=BB[bi][0:64, (h % 8) * 128:(h % 8 + 1) * 128], in_=qb[:, h * 64:(h + 1) * 64], identity=idb[:]),
                 reads=["qb", "idb"], writes=[f"B{bi}"])
        qd = qT[qslot]
        S.act(lambda e: e.copy(out=qd[:, 0:1024], in_=BB[1][0:64, 0:1024]), reads=["B1"], writes=[f"qT{qslot}"])
        S.dve(lambda e: e.tensor_copy(out=qd[:, 1024:2048], in_=BB[2][0:64, 0:1024]), reads=["B2", f"qT{qslot}"], writes=[f"qT{qslot}"])
        S.act(lambda e: e.copy(out=kT_dst[:], in_=BB[3][0:64, 0:512]), reads=["B3"], writes=[kkey])

    def attn(sx, qslot, keys, mod, dst_rows, dkey, hslot):
        qd = qT[qslot]
        qk_ = f"qT{qslot}"
        cnt = [0]
        for g in range(4):
            nk = len(keys)
            for idx, (kt_, v_, kkey, vkey, m_, mkey) in enumerate(keys):
                c = cnt[0]
                cnt[0] += 1
                sb = 4 + (c % 2)
                sbk = f"B{sb}"
                S.pe(lambda e, kt_=kt_, sb=sb, g=g, m_=m_: e.matmul(B[sb][:], lhsT=kt_[:, g * 128:(g + 1) * 128], rhs=qd[:, g * 512:(g + 1) * 512], start=True, stop=(m_ is None)),
                     reads=[kkey, qk_], writes=[sbk])
                if m_ is not None:
                    S.pe(lambda e, sb=sb, m_=m_: e.matmul(B[sb][:], lhsT=idb[:], rhs=m_[:], start=False, stop=True), reads=["idb", mkey], writes=[sbk])
                pslot = c % NP
                p_ = pT[pslot]
                pk = f"pT{pslot}"
                S.act(lambda e, sb=sb, p_=p_: e.activation(out=p_[:], in_=B[sb][:], func=AF.Exp, scale=0.125), reads=[sbk], writes=[pk])
                S.pe(lambda e, v_=v_, g=g, p_=p_, idx=idx, nk=nk: e.matmul(B[6][0:64, :], lhsT=v_[:, g * 64:(g + 1) * 64], rhs=p_[:], start=(idx == 0), stop=(idx == nk - 1)),
                     reads=[vkey, pk], writes=["B6"])
                S.pe(lambda e, p_=p_, idx=idx: e.matmul(B[7][0:64, :], lhsT=onesk[:], rhs=p_[:], start=(idx == 0), stop=False), reads=["onesk", pk], writes=["B7"])
            S.pe(lambda e, g=g: e.matmul(B[7][0:64, :], lhsT=onesk[0:1, :], rhs=sinkrow[0:1, g * 512:(g + 1) * 512], start=False, stop=True), reads=["onesk", "sinkrow"], writes=["B7"])
            S.dve(lambda e: e.reciprocal(out=rden[:], in_=B[7][0:64, :]), reads=["B7"], writes=["rden"])
            S.dve(lambda e, g=g: e.tensor_tensor(out=attnT[:, g * 512:(g + 1) * 512], in0=B[6][0:64, :], in1=rden[:], op=ALU.mult), reads=["B6", "rden", "attnT"], writes=["attnT"])
        h_ = ht[hslot]
        hk = f"ht{hslot}"
        for half in range(2):
            for h in range(16):
                S.pe(lambda e, half=half, h=h: e.matmul(B[half][:], lhsT=attnT[:, h * 128:(h + 1) * 128], rhs=wo[:, h, half * 512:(half + 1) * 512], start=(h == 0), stop=(h == 15)),
                     reads=["attnT", "wo"], writes=[f"B{half}"])
            S.dve(lambda e, half=half: e.tensor_tensor(out=h_[:, half * 512:(half + 1) * 512], in0=B[half][:], in1=mod[:, 2 * D + half * 512:2 * D + (half + 1) * 512], op=ALU.mult),
                  reads=[f"B{half}", "mod", hk], writes=[hk])
        S.dve(lambda e: e.tensor_tensor(out=h_[:], in0=h_[:], in1=xt[sx][:], op=ALU.add), reads=[hk, f"xt{sx}"], writes=[hk])
        S.dma("sp", dst_rows, h_[:], reads=[hk], writes=[dkey])

    for c in range(2):
        pre(k.ctx[c * 128:(c + 1) * 128, :], modc, None, c, c, kTc[c], vtc[c], f"kTc{c}", f"vtc{c}")
    ckeys = [(kTc[c], vtc[c], f"kTc{c}", f"vtc{c}", None, None) for c in range(2)]
    for c in range(2):
        attn(c, c, ckeys, modc, k.h1c[c * 128:(c + 1) * 128, :], "h1c", c)
    nt = n_lat_tiles
    nq = min(NQ_LAT, nt - 1) if nt > 1 else 1

    def pre_lat(i):
        pre(k.x[i * 128:(i + 1) * 128, :], modl, (i, i % 2), i % NXT, i % NQ, kT[i % NKV], vt[i % NKV], f"kT{i % NKV}", f"vt{i % NKV}")

    pre_lat(0)
    for i in range(nq):
        if i + 1 < nt:
            pre_lat(i + 1)
        keys = []
        if i >= 1:
            j = (i - 1) % NKV
            keys.append((kT[j], vt[j], f"kT{j}", f"vt{j}", maskp, "maskp"))
        j = i % NKV
        keys.append((kT[j], vt[j], f"kT{j}", f"vt{j}", None, None))
        if i + 1 < nt:
            j = (i + 1) % NKV
            keys.append((kT[j], vt[j], f"kT{j}", f"vt{j}", maskn, "maskn"))
        keys += ckeys
        attn(i % NXT, i % NQ, keys, modl, k.h1[i * 128:(i + 1) * 128, :], "h1", i % 2)


MB = 512
BIG = 1.0e9


def moe_nblocks(ntiles):
    nslot = ntiles * 128 * 2
    return (nslot + 32 * (MB - 1)) // MB


def phase_moe(k, layer, toks):
    S, nc = k.S, k.nc
    B, BB = k.banks, k.bbf
    L = layer
    NTt = len(toks)
    NB = moe_nblocks(NTt)
    J = (NTt * 128 + MB - 1) // MB
    assert J <= 16 and NB <= 64
    pfx = f"m{L}_"
    with ExitStack() as stp:
        TT = lambda name, shape, dt: stp.enter_context(nc.sbuf_tensor(pfx + name, list(shape), dt))
        idf = TT("idf", [128, 128], F32)
        idb = TT("idb", [128, 128], BF16)
        ustr = TT("ustr", [128, 128], BF16)
        onesb = TT("onesb", [128, 128], BF16)
        mc = TT("mc", [128, 96], F32)
        wr = TT("wr", [128, 8, 36], F32)
        brb = TT("brb", [128, 36], F32)
        S.dma("sp", idf[:], k.consts[:, 0:128], writes=["idf"])
        S.dma("pool", idb[:], k.consts[:, 0:128], writes=["idb"])
        S.dma("pool", ustr[:], k.consts[:, 128:256], writes=["ustr"])
        S.dma("sp", mc[:], k.mconst, writes=["mc"])
        S.dma("sp", wr[:], k.w_router[L].rearrange("(k p) n -> p k n", p=128), writes=["wr"])
        S.dma("sp", brb[:], k.b_router[L].partition_broadcast(128), writes=["brb"])
        S.pool(lambda e: e.memset(onesb[:], 1.0), writes=["onesb"])
        oh_all = TT("oh_all", [128, 2, NTt, 32], F32)
        r_all = TT("r_all", [128, 2, NTt], F32)
        g_all = TT("g_all", [128, 2, NTt], F32)
        base = TT("base", [128, 32], F32)
        S.pool(lambda e: e.memset(base[:], 0.0), writes=["base"])
        with ExitStack() as st1:
            T1 = lambda name, shape, dt: st1.enter_context(nc.sbuf_tensor(pfx + name, list(shape), dt))
            xt = [T1(f"xt{i}", [128, D], F32) for i in range(2)]
            junk = T1("junk", [128, D], F32)
            ssq = T1("ssq", [128, 1], F32)
            rstd = T1("rstd", [128, 1], F32)
            mf = T1("mf", [128, D], F32)
            mb = [T1(f"mb{i}", [128, D], BF16) for i in range(2)]
            mT = T1("mT", [128, D], F32)
            lg = T1("lg", [128, 36], F32)
            sm = T1("sm", [128, 16], F32)
            ohg = T1("ohg", [128, 4], F32)
            pen = T1("pen", [128, 4], F32)
            lem = T1("lem", [128, 32], F32)
            top8 = T1("top8", [128, 8], F32)
            msk = T1("msk", [128, 32], F32)
            mskb = T1("mskb", [128, 32], BF16)
            rk = T1("rk", [128, 32], F32)
            tmp = T1("tmp", [128, 32], F32)
            junk4 = T1("junk4", [128, 4], F32)
            for n, (src, skey, mod, dst, dkey) in enumerate(toks):
                x_ = xt[n % 2]
                xk = f"mxt{n % 2}"
                S.dma("sp", x_[:], src, reads=[skey], writes=[xk])
                S.pool(lambda e: e.memset(ssq[:], 0.0), writes=["ssq"])
                S.act(lambda e, x_=x_: e.activation(out=junk[:], in_=x_[:], func=AF.Square, accum_out=ssq[:]), reads=[xk], writes=["junk", "ssq"])
                rstd_from_ssq(k, ssq[:], rstd[:], 1.0 / D, "ssq", "rstd")
                S.dve(lambda e, x_=x_, mod=mod: e.scalar_tensor_tensor(out=mf[:], in0=x_[:], scalar=rstd[:, 0:1], in1=mod[:, 4 * D:5 * D], op0=ALU.mult, op1=ALU.mult),
                      reads=[xk, "rstd"], writes=["mf"])
                S.dve(lambda e, mod=mod: e.tensor_tensor(out=mf[:], in0=mf[:], in1=mod[:, 3 * D:4 * D], op=ALU.add), reads=["mf"], writes=["mf"])
                m_ = mb[n % 2]
                mk = f"mb{n % 2}"
                S.act(lambda e, m_=m_: e.copy(out=m_[:], in_=mf[:]), reads=["mf"], writes=[mk])
                S.dma("sp", k.mbuf[n * 128:(n + 1) * 128, :], m_[:], reads=[mk], writes=[f"mbuf{n}"])
                for half in range(2):
                    for kk in range(4):
                        kq = half * 4 + kk
                        S.pe(lambda e, half=half, kk=kk, kq=kq: e.transpose(out=B[half][:, kk * 128:(kk + 1) * 128], in_=mf[:, kq * 128:(kq + 1) * 128], identity=idf[:]),
                             reads=["mf", "idf"], writes=[f"B{half}"])
                S.act(lambda e: e.copy(out=mT[:, 0:512], in_=B[0][:]), reads=["B0"], writes=["mT"])
                S.dve(lambda e: e.tensor_copy(out=mT[:, 512:1024], in_=B[1][:]), reads=["B1", "mT"], writes=["mT"])
                for kk in range(8):
                    S.pe(lambda e, kk=kk: e.matmul(B[2][:, 0:36], lhsT=mT[:, kk * 128:(kk + 1) * 128], rhs=wr[:, kk, :], start=(kk == 0), stop=(kk == 7)),
                         reads=["mT", "wr"], writes=["B2"])
                S.dve(lambda e: e.tensor_tensor(out=lg[:], in0=B[2][:, 0:36], in1=brb[:], op=ALU.add), reads=["B2", "brb"], writes=["lg"])
                S.dve(lambda e: e.reduce_max(out=sm[:, 0:1], in_=lg[:, 0:4], axis=AX.X), reads=["lg"], writes=["sm"])
                S.dve(lambda e: e.tensor_scalar(out=ohg[:], in0=lg[:, 0:4], scalar1=sm[:, 0:1], scalar2=None, op0=ALU.is_ge), reads=["lg", "sm"], writes=["ohg"])
                S.dve(lambda e: e.tensor_scalar(out=sm[:, 1:2], in0=sm[:, 0:1], scalar1=-1.0, scalar2=None, op0=ALU.mult), reads=["sm"], writes=["sm"])
                S.pool(lambda e: e.memset(sm[:, 2:3], 0.0), reads=["sm"], writes=["sm"])
                S.act(lambda e: e.activation(out=junk4[:], in_=lg[:, 0:4], func=AF.Exp, bias=sm[:, 1:2], scale=1.0, accum_out=sm[:, 2:3]), reads=["lg", "sm"], writes=["junk4", "sm"])
                S.dve(lambda e: e.reciprocal(out=sm[:, 3:4], in_=sm[:, 2:3]), reads=["sm"], writes=["sm"])
                S.dve(lambda e: e.tensor_scalar(out=pen[:], in0=ohg[:], scalar1=-1.0, scalar2=BIG, op0=ALU.add, op1=ALU.mult), reads=["ohg"], writes=["pen"])
                S.dve(lambda e: e.tensor_tensor(out=lem[:].rearrange("p (g j) -> p g j", j=8), in0=lg[:, 4:36].rearrange("p (g j) -> p g j", j=8),
                                                in1=bc_free(pen[:], [128, 4, 8]), op=ALU.add), reads=["lg", "pen"], writes=["lem"])
                S.dve(lambda e: e.max(out=top8[:], in_=lem[:]), reads=["lem"], writes=["top8"])
                oh1 = oh_all[:, 0, n, :]
                oh2 = oh_all[:, 1, n, :]
                S.dve(lambda e, oh1=oh1: e.tensor_scalar(out=oh1, in0=lem[:], scalar1=top8[:, 0:1], scalar2=None, op0=ALU.is_equal), reads=["lem", "top8"], writes=[f"oh{n}"])
                S.dve(lambda e, oh2=oh2: e.tensor_scalar(out=oh2, in0=lem[:], scalar1=top8[:, 1:2], scalar2=None, op0=ALU.is_equal), reads=["lem", "top8", f"oh{n}"], writes=[f"oh{n}"])
                S.dve(lambda e: e.tensor_tensor(out=sm[:, 4:5], in0=top8[:, 1:2], in1=top8[:, 0:1], op=ALU.subtract), reads=["top8", "sm"], writes=["sm"])
                S.act(lambda e: e.activation(out=sm[:, 5:6], in_=sm[:, 4:5], func=AF.Exp), reads=["sm"], writes=["sm"])
                S.dve(lambda e: e.tensor_scalar(out=sm[:, 6:7], in0=sm[:, 5:6], scalar1=1.0, scalar2=None, op0=ALU.add), reads=["sm"], writes=["sm"])
                S.dve(lambda e: e.reciprocal(out=sm[:, 7:8], in_=sm[:, 6:7]), reads=["sm"], writes=["sm"])
                S.dve(lambda e, n=n: e.tensor_tensor(out=g_all[:, 0, n:n + 1], in0=sm[:, 7:8], in1=sm[:, 3:4], op=ALU.mult), reads=["sm"], writes=[f"g{n}"])
                S.dve(lambda e, n=n: e.tensor_tensor(out=g_all[:, 1, n:n + 1], in0=g_all[:, 0, n:n + 1], in1=sm[:, 5:6], op=ALU.mult), reads=["sm", f"g{n}"], writes=[f"g{n}"])
                S.dve(lambda e, oh1=oh1, oh2=oh2: e.tensor_tensor(out=msk[:], in0=oh1, in1=oh2, op=ALU.add), reads=[f"oh{n}"], writes=["msk"])
                S.act(lambda e: e.copy(out=mskb[:], in_=msk[:]), reads=["msk"], writes=["mskb"])
                S.pe(lambda e: e.matmul(B[3][:, 0:32], lhsT=ustr[:], rhs=mskb[:], start=True, stop=True), reads=["ustr", "mskb"], writes=["B3"])
                S.pe(lambda e: e.matmul(B[3][:, 32:64], lhsT=onesb[:], rhs=mskb[:], start=True, stop=True), reads=["onesb", "mskb"], writes=["B3"])
                S.dve(lambda e: e.tensor_tensor(out=rk[:], in0=B[3][:, 0:32], in1=base[:], op=ALU.add), reads=["B3", "base"], writes=["rk"])
                S.dve(lambda e: e.tensor_tensor(out=base[:], in0=base[:], in1=B[3][:, 32:64], op=ALU.add), reads=["B3", "base"], writes=["base"])
                for j, oh in enumerate((oh1, oh2)):
                    S.dve(lambda e, oh=oh: e.tensor_tensor(out=tmp[:], in0=rk[:], in1=oh, op=ALU.mult), reads=["rk", f"oh{n}"], writes=["tmp"])
                    S.dve(lambda e, j=j, n=n: e.reduce_sum(out=r_all[:, j, n:n + 1], in_=tmp[:], axis=AX.X), reads=["tmp"], writes=[f"r{n}"])
            S.barrier(k.bar[:])
        pos_i = TT("pos_i", [128, 2, NTt], I32)
        idx_in = TT("idx_in", [128, NB, 8], I32)
        idx_out = TT("idx_out", [128, NB, 4], I32)
        allk = [f"oh{n}" for n in range(NTt)] + [f"r{n}" for n in range(NTt)] + [f"g{n}" for n in range(NTt)]
        with ExitStack() as st2:
            T2 = lambda name, shape, dt: st2.enter_context(nc.sbuf_tensor(pfx + name, list(shape), dt))
            cmp = T2("cmp", [128, 32, J], F32)
            nblk = T2("nblk", [128, 32], F32)
            ends = T2("ends", [128, 32], F32)
            sslot = T2("sslot", [128, 32], F32)
            cmp2 = T2("cmp2", [128, NB, 32], F32)
            blke = T2("blke", [128, NB], F32)
            fin = T2("fin", [128, NB, 8], F32)
            fout = T2("fout", [128, NB, 4], F32)
            big = T2("big", [128, NTt, 32], F32)
            posf = T2("posf", [128, 2, NTt], F32)
            S.dve(lambda e: e.tensor_tensor(out=cmp[:], in0=bc_free(base[:], [128, 32, J]), in1=bc_mid(mc[:, 0:J], [128, 32, J]), op=ALU.is_gt), reads=["base", "mc"], writes=["cmp"])
            S.dve(lambda e: e.reduce_sum(out=nblk[:], in_=cmp[:], axis=AX.X), reads=["cmp"], writes=["nblk"])
            S.dve(lambda e: e.tensor_copy(out=ends[:, 0:1], in_=nblk[:, 0:1]), reads=["nblk"], writes=["ends"])
            for e_ in range(1, 32):
                S.dve(lambda e, e_=e_: e.tensor_tensor(out=ends[:, e_:e_ + 1], in0=ends[:, e_ - 1:e_], in1=nblk[:, e_:e_ + 1], op=ALU.add), reads=["ends", "nblk"], writes=["ends"])
            S.dve(lambda e: e.tensor_tensor(out=sslot[:], in0=ends[:], in1=nblk[:], op=ALU.subtract), reads=["ends", "nblk"], writes=["sslot"])
            S.dve(lambda e: e.tensor_scalar(out=sslot[:], in0=sslot[:], scalar1=float(MB), scalar2=None, op0=ALU.mult), reads=["sslot"], writes=["sslot"])
            S.dve(lambda e: e.tensor_tensor(out=cmp2[:], in0=bc_mid(ends[:], [128, NB, 32]), in1=bc_free(mc[:, 32:32 + NB], [128, NB, 32]), op=ALU.is_le), reads=["ends", "mc"], writes=["cmp2"])
            S.dve(lambda e: e.reduce_sum(out=blke[:], in_=cmp2[:], axis=AX.X), reads=["cmp2"], writes=["blke"])
            S.dve(lambda e: e.tensor_scalar(out=blke[:], in0=blke[:], scalar1=31.0, scalar2=None, op0=ALU.min), reads=["blke"], writes=["blke"])
            S.dve(lambda e: e.tensor_scalar(out=fin[:], in0=bc_free(blke[:], [128, NB, 8]), scalar1=1024.0, scalar2=None, op0=ALU.mult), reads=["blke"], writes=["fin"])
            S.dve(lambda e: e.tensor_tensor(out=fin[:], in0=fin[:], in1=bc_mid(mc[:, 16:24], [128, NB, 8]), op=ALU.add), reads=["fin", "mc"], writes=["fin"])
            S.dve(lambda e: e.tensor_copy(out=idx_in[:], in_=fin[:]), reads=["fin"], writes=["idx_in"])
            S.dve(lambda e: e.tensor_scalar(out=fout[:], in0=bc_free(blke[:], [128, NB, 4]), scalar1=512.0, scalar2=None, op0=ALU.mult), reads=["blke"], writes=["fout"])
            S.dve(lambda e: e.tensor_tensor(out=fout[:], in0=fout[:], in1=bc_mid(mc[:, 24:28], [128, NB, 4]), op=ALU.add), reads=["fout", "mc"], writes=["fout"])
            S.dve(lambda e: e.tensor_copy(out=idx_out[:], in_=fout[:]), reads=["fout"], writes=["idx_out"])
            for j in range(2):
                S.dve(lambda e, j=j: e.tensor_tensor(out=big[:], in0=oh_all[:, j, :, :], in1=bc_mid(sslot[:], [128, NTt, 32]), op=ALU.mult), reads=allk + ["sslot"], writes=["big"])
                S.dve(lambda e, j=j: e.reduce_sum(out=posf[:, j, :], in_=big[:], axis=AX.X), reads=["big"], writes=["posf"])
            S.dve(lambda e: e.tensor_tensor(out=posf[:], in0=posf[:], in1=r_all[:], op=ALU.add), reads=["posf"] + allk, writes=["posf"])
            S.dve(lambda e: e.tensor_copy(out=pos_i[:], in_=posf[:]), reads=["posf"], writes=["pos_i"])
            S.barrier(k.bar[:])
        with ExitStack() as st3:
            T3 = lambda name, shape, dt: st3.enter_context(nc.sbuf_tensor(pfx + name, list(shape), dt))
            ml = [T3(f"ml{i}", [128, D], BF16) for i in range(3)]
            for n in range(NTt):
                m_ = ml[n % 3]
                mk = f"ml{n % 3}"
                S.dma("sp", m_[:], k.mbuf[n * 128:(n + 1) * 128, :], reads=[f"mbuf{n}"], writes=[mk])
                for j in range(2):
                    S.op("pool", lambda e, m_=m_, j=j, n=n: e.indirect_dma_start(out=k.xs, out_offset=bass.IndirectOffsetOnAxis(ap=pos_i[:, j, n:n + 1], axis=0), in_=m_[:], in_offset=None),
                         reads=[mk, "pos_i"], writes=[f"xs_{n}_{j}"], dma=True)
            S.barrier(k.bar[:])
        xs_keys = [f"xs_{n}_{j}" for n in range(NTt) for j in range(2)]
        with ExitStack() as st4:
            T4 = lambda name, shape, dt: st4.enter_context(nc.sbuf_tensor(pfx + name, list(shape), dt))
            win = [T4(f"win{i}", [128, 8, 1024], BF16) for i in range(2)]
            wout = [T4(f"wout{i}", [128, 4, 1024], BF16) for i in range(2)]
            xsl = [T4(f"xsl{i}", [128, D], BF16) for i in range(4)]
            xsT = [T4(f"xsT{i}", [128, 8, MB], BF16) for i in range(2)]
            sg = [T4(f"sg{i}", [128, MB], F32) for i in range(2)]
            actt = [T4(f"act{i}", [128, 4, MB], BF16) for i in range(2)]
            yt = [T4(f"yt{i}", [128, D], F32) for i in range(2)]
            w_in_rows = k.moe_w_in[k.lmap[L]].rearrange("e d f -> (e d) f")
            w_out_rows = k.moe_w_out[k.lmap[L]].rearrange("e d f -> (e d) f")
            nsl = MB // 128
            xcnt = [0]
            ycnt = [0]
            for b in range(NB):
                ws = b % 2
                wi, wo_ = win[ws], wout[ws]
                wik, wok = f"win{ws}", f"wout{ws}"
                for kk in range(8):
                    S.op("pool", lambda e, wi=wi, kk=kk, b=b: e.indirect_dma_start(out=wi[:, kk, :], out_offset=None, in_=w_in_rows,
                                                                               in_offset=bass.IndirectOffsetOnAxis(ap=idx_in[:, b, kk:kk + 1], axis=0)),
                         reads=["idx_in"], writes=[f"{wik}_{kk}"], dma=True)
                for ff in range(4):
                    S.op("pool", lambda e, wo_=wo_, ff=ff, b=b: e.indirect_dma_start(out=wo_[:, ff, :], out_offset=None, in_=w_out_rows,
                                                                                 in_offset=bass.IndirectOffsetOnAxis(ap=idx_out[:, b, ff:ff + 1], axis=0)),
                         reads=["idx_out"], writes=[f"{wok}_{ff}"], dma=True)
                xT_ = xsT[b % 2]
                xTk = f"xsT{b % 2}"
                for s in range(nsl):
                    c = xcnt[0]
                    xcnt[0] += 1
                    xl = xsl[c % 4]
                    xlk = f"xsl{c % 4}"
                    S.dma("sp", xl[:], k.xs[b * MB + s * 128:b * MB + (s + 1) * 128, :], reads=xs_keys, writes=[xlk])
                    tb = c % 2
                    for kk in range(8):
                        S.pe(lambda e, tb=tb, kk=kk, xl=xl: e.transpose(out=BB[tb][:, kk * 128:(kk + 1) * 128], in_=xl[:, kk * 128:(kk + 1) * 128], identity=idb[:]),
                             reads=[xlk, "idb"], writes=[f"B{tb}"])
                    S.act(lambda e, tb=tb, xT_=xT_, s=s: e.copy(out=xT_[:, :, s * 128:(s + 1) * 128], in_=BB[tb][:, 0:D].rearrange("p (k t) -> p k t", t=128)),
                          reads=[f"B{tb}"], writes=[xTk])
                a_ = actt[b % 2]
                ak = f"act{b % 2}"
                for fp in range(4):
                    gb, ub = 2 + (fp % 2) * 2, 3 + (fp % 2) * 2
                    for (bank, f) in ((gb, fp), (ub, fp + 4)):
                        for kk in range(8):
                            S.pe(lambda e, bank=bank, f=f, kk=kk, wi=wi, xT_=xT_: e.matmul(B[bank][:, 0:MB], lhsT=wi[:, kk, f * 128:(f + 1) * 128], rhs=xT_[:, kk, :], start=(kk == 0), stop=(kk == 7)),
                                 reads=[f"{wik}_{kk}", xTk], writes=[f"B{bank}"])
                    sg_ = sg[fp % 2]
                    sgk = f"sg{fp % 2}"
                    S.act(lambda e, gb=gb, sg_=sg_: e.activation(out=sg_[:], in_=B[gb][:, 0:MB], func=AF.Silu), reads=[f"B{gb}"], writes=[sgk])
                    S.dve(lambda e, ub=ub, sg_=sg_, a_=a_, fp=fp: e.tensor_tensor(out=a_[:, fp, :], in0=sg_[:], in1=B[ub][:, 0:MB], op=ALU.mult), reads=[sgk, f"B{ub}", ak], writes=[ak])
                for s in range(nsl):
                    c = ycnt[0]
                    ycnt[0] += 1
                    y_ = yt[c % 2]
                    yk = f"yt{c % 2}"
                    for half in range(2):
                        bank = 6 + half
                        for fp in range(4):
                            S.pe(lambda e, bank=bank, fp=fp, s=s, half=half, a_=a_, wo_=wo_: e.matmul(B[bank][:], lhsT=a_[:, fp, s * 128:(s + 1) * 128], rhs=wo_[:, fp, half * 512:(half + 1) * 512], start=(fp == 0), stop=(fp == 3)),
                                 reads=[ak, f"{wok}_{fp}"], writes=[f"B{bank}"])
                    S.act(lambda e, y_=y_: e.copy(out=y_[:, 0:512], in_=B[6][:]), reads=["B6", yk], writes=[yk])
                    S.dve(lambda e, y_=y_: e.tensor_copy(out=y_[:, 512:1024], in_=B[7][:]), reads=["B7", yk], writes=[yk])
                    S.dma("sp", k.ys[b * MB + s * 128:b * MB + (s + 1) * 128, :], y_[:], reads=[yk], writes=[f"ys{b}_{s}"])
            S.barrier(k.bar[:])
        ys_keys = [f"ys{b}_{s}" for b in range(NB) for s in range(MB // 128)]
        with ExitStack() as st5:
            T5 = lambda name, shape, dt: st5.enter_context(nc.sbuf_tensor(pfx + name, list(shape), dt))
            y1 = [T5(f"y1_{i}", [128, D], F32) for i in range(2)]
            y2 = [T5(f"y2_{i}", [128, D], F32) for i in range(2)]
            hx = [T5(f"hx{i}", [128, D], F32) for i in range(2)]
            for n, (src, skey, mod, dst, dkey) in enumerate(toks):
                a, b_, h_ = y1[n % 2], y2[n % 2], hx[n % 2]
                ak, bk, hk = f"y1_{n % 2}", f"y2_{n % 2}", f"hx{n % 2}"
                S.dma("sp", h_[:], src, reads=[skey], writes=[hk])
                S.op("pool", lambda e, a=a, n=n: e.indirect_dma_start(out=a[:], out_offset=None, in_=k.ys, in_offset=bass.IndirectOffsetOnAxis(ap=pos_i[:, 0, n:n + 1], axis=0)),
                     reads=ys_keys + ["pos_i"], writes=[ak], dma=True)
                S.op("pool", lambda e, b_=b_, n=n: e.indirect_dma_start(out=b_[:], out_offset=None, in_=k.ys, in_offset=bass.IndirectOffsetOnAxis(ap=pos_i[:, 1, n:n + 1], axis=0)),
                     reads=ys_keys + ["pos_i"], writes=[bk], dma=True)
                S.dve(lambda e, a=a, n=n: e.tensor_scalar(out=a[:], in0=a[:], scalar1=g_all[:, 0, n:n + 1], scalar2=None, op0=ALU.mult), reads=[ak, f"g{n}"], writes=[ak])
                S.dve(lambda e, a=a, b_=b_, n=n: e.scalar_tensor_tensor(out=a[:], in0=b_[:], scalar=g_all[:, 1, n:n + 1], in1=a[:], op0=ALU.mult, op1=ALU.add), reads=[ak, bk, f"g{n}"], writes=[ak])
                S.pool(lambda e, a=a, mod=mod: e.tensor_tensor(out=a[:], in0=a[:], in1=mod[:, 5 * D:6 * D], op=ALU.mult), reads=[ak], writes=[ak])
                S.dve(lambda e, a=a, h_=h_: e.tensor_tensor(out=h_[:], in0=h_[:], in1=a[:], op=ALU.add), reads=[ak, hk], writes=[hk])
                S.dma("sp", dst, h_[:], reads=[hk], writes=[dkey])
        S.barrier(k.bar[:])


DN_C = 64
NTOKP = LCTX + NOWN


def phase_dn_pre(k):
    S, nc = k.S, k.nc
    B, BB = k.banks, k.bbf
    modl, modc = k.modl, k.modc
    with ExitStack() as stp:
        TT = lambda name, shape, dt: stp.enter_context(nc.sbuf_tensor("dp_" + name, list(shape), dt))
        NCOL = LCTX + NOWN + 128
        aT_all = TT("aT_all", [128, 8, NCOL], BF16)
        idb = TT("idb", [128, 128], BF16)
        one1 = TT("one1", [128, 1], F32)
        S.dma("pool", idb[:], k.consts[:, 0:128], writes=["idb"])
        S.pool(lambda e: e.memset(one1[:], 1.0), writes=["one1"])
        with ExitStack() as st1:
            T1 = lambda name, shape, dt: st1.enter_context(nc.sbuf_tensor("dp_" + name, list(shape), dt))
            wz = T1("wz", [128, 8, 1024 + 32], BF16)
            S.dma("pool", wz[:], k.dn_w_in.rearrange("(k p) n -> p k n", p=128)[:, :, 3072:4128], writes=["wz"])
            dtb = T1("dtb", [128, 16], F32)
            alg = T1("alg", [128, 16], F32)
            nega = T1("nega", [128, 16], F32)
            S.dma("sp", dtb[:], k.dn_dtb.partition_broadcast(128), writes=["dtb"])
            S.dma("sp", alg[:], k.dn_alog.partition_broadcast(128), writes=["alg"])
            S.act(lambda e: e.activation(out=nega[:], in_=alg[:], func=AF.Exp), reads=["alg"], writes=["nega"])
            S.dve(lambda e: e.tensor_scalar(out=nega[:], in0=nega[:], scalar1=-1.0, scalar2=None, op0=ALU.mult), reads=["nega"], writes=["nega"])
            xt = [T1(f"xt{i}", [128, D], F32) for i in range(2)]
            junk = T1("junk", [128, D], F32)
            ssq = T1("ssq", [128, 1], F32)
            rstd = T1("rstd", [128, 1], F32)
            af = T1("af", [128, D], F32)
            ab = T1("ab", [128, D], BF16)
            zt = [T1(f"zt{i}", [128, D], BF16) for i in range(2)]
            gbt = [T1(f"gbt{i}", [128, 32], F32) for i in range(2)]
            tiles = [(k.h2c[c * 128:(c + 1) * 128, :], "h2c", modc, c * 128, None, c * 128) for c in range(2)]
            tiles += [(k.h2[i * 128:(i + 1) * 128, :], "h2", modl, LCTX + i * 128, (i if i < 32 else None), (LCTX + i * 128 if i < 32 else None)) for i in range(33)]
            for n, (src, skey, mod, col0, zrow, gbrow) in enumerate(tiles):
                x_ = xt[n % 2]
                xk = f"dxt{n % 2}"
                S.dma("sp", x_[:], src, reads=[skey], writes=[xk])
                S.pool(lambda e: e.memset(ssq[:], 0.0), writes=["ssq"])
                S.act(lambda e, x_=x_: e.activation(out=junk[:], in_=x_[:], func=AF.Square, accum_out=ssq[:]), reads=[xk], writes=["junk", "ssq"])
                rstd_from_ssq(k, ssq[:], rstd[:], 1.0 / D, "ssq", "rstd")
                S.dve(lambda e, x_=x_, mod=mod: e.scalar_tensor_tensor(out=af[:], in0=x_[:], scalar=rstd[:, 0:1], in1=mod[:, 1 * D:2 * D], op0=ALU.mult, op1=ALU.mult),
                      reads=[xk, "rstd"], writes=["af"])
                S.dve(lambda e, mod=mod: e.tensor_tensor(out=ab[:], in0=af[:], in1=mod[:, 0:D], op=ALU.add), reads=["af"], writes=["ab"])
                for kk in range(8):
                    S.pe(lambda e, kk=kk: e.transpose(out=BB[0][:, kk * 128:(kk + 1) * 128], in_=ab[:, kk * 128:(kk + 1) * 128], identity=idb[:]), reads=["ab", "idb"], writes=["B0"])
                S.act(lambda e, col0=col0: e.copy(out=aT_all[:, :, col0:col0 + 128], in_=BB[0][:, 0:D].rearrange("p (k t) -> p k t", t=128)), reads=["B0"], writes=[f"aT{n}"])
                if zrow is not None:
                    z_ = zt[n % 2]
                    zk = f"zt{n % 2}"
                    for half in range(2):
                        for kk in range(8):
                            S.pe(lambda e, half=half, kk=kk, col0=col0: e.matmul(B[1 + half][:], lhsT=aT_all[:, kk, col0:col0 + 128], rhs=wz[:, kk, half * 512:(half + 1) * 512], start=(kk == 0), stop=(kk == 7)),
                                 reads=[f"aT{n}", "wz"], writes=[f"B{1 + half}"])
                        S.act(lambda e, half=half, z_=z_: e.activation(out=z_[:, half * 512:(half + 1) * 512], in_=B[1 + half][:], func=AF.Silu), reads=[f"B{1 + half}", zk], writes=[zk])
                    S.dma("sp", k.zs[zrow * 128:(zrow + 1) * 128, :], z_[:], reads=[zk], writes=["zs"])
                if gbrow is not None:
                    g_ = gbt[n % 2]
                    gk = f"gbt{n % 2}"
                    for kk in range(8):
                        S.pe(lambda e, kk=kk, col0=col0: e.matmul(B[3][:, 0:32], lhsT=aT_all[:, kk, col0:col0 + 128], rhs=wz[:, kk, 1024:1056], start=(kk == 0), stop=(kk == 7)),
                             reads=[f"aT{n}", "wz"], writes=["B3"])
                    S.dve(lambda e, g_=g_: e.tensor_tensor(out=g_[:, 0:16], in0=B[3][:, 0:16], in1=dtb[:], op=ALU.add), reads=["B3", "dtb", gk], writes=[gk])
                    S.act(lambda e, g_=g_: e.activation(out=g_[:, 0:16], in_=g_[:, 0:16], func=AF.Exp), reads=[gk], writes=[gk])
                    S.act(lambda e, g_=g_: e.activation(out=g_[:, 0:16], in_=g_[:, 0:16], func=AF.Ln, bias=one1[:], scale=1.0), reads=[gk, "one1"], writes=[gk])
                    S.dve(lambda e, g_=g_: e.tensor_tensor(out=g_[:, 0:16], in0=g_[:, 0:16], in1=nega[:], op=ALU.mult), reads=[gk, "nega"], writes=[gk])
                    S.act(lambda e, g_=g_: e.activation(out=g_[:, 16:32], in_=B[3][:, 16:32], func=AF.Sigmoid), reads=["B3", gk], writes=[gk])
                    S.dma("sp", k.gb[gbrow:gbrow + 128, :], g_[:], reads=[gk], writes=["gb"])
            S.barrier(k.bar[:])
        aTkeys = [f"aT{n}" for n in range(35)]
        with ExitStack() as st2:
            T2 = lambda name, shape, dt: st2.enter_context(nc.sbuf_tensor("dp_" + name, list(shape), dt))
            HN = 2048
            cw = T2("cw", [128, 24, 5], F32)
            S.dma("sp", cw[:], k.dn_conv.rearrange("p (c j) -> p c j", j=5), writes=["cw"])
            wc = [T2(f"wc{i}", [128, 8, 128], BF16) for i in range(2)]
            pc = [T2(f"pc{i}", [128, HN + 4], F32) for i in range(2)]
            acc = [T2(f"acc{i}", [128, HN], F32) for i in range(2)]
            sq = T2("sq", [128, HN], BF16)
            onesb2 = T2("onesb2", [128, 128], BF16)
            S.pool(lambda e: e.memset(onesb2[:], 1.0), writes=["onesb2"])
            rs = T2("rs", [128, 512], F32)
            ob = [T2(f"ob{i}", [128, HN], BF16) for i in range(2)]
            wv = k.dn_w_in.rearrange("(k p) n -> p k n", p=128)
            cnt = 0
            for ch in range(24):
                w_ = wc[ch % 2]
                wk = f"wc{ch % 2}"
                S.dma("pool", w_[:], wv[:, :, ch * 128:(ch + 1) * 128], writes=[wk])
                segs = [(0, LCTX, True, True, 0), (LCTX, HN, True, False, LCTX), (LCTX + HN, HN, False, False, LCTX + HN)]
                for (c0, N, lz, rz, dcol) in segs:
                    p_ = pc[cnt % 2]
                    pk = f"pc{cnt % 2}"
                    a_ = acc[cnt % 2]
                    ak = f"acc{cnt % 2}"
                    o_ = ob[cnt % 2]
                    ok = f"ob{cnt % 2}"
                    cnt += 1
                    lo = c0 - (0 if lz else 2)
                    hi = c0 + N + (0 if rz else 2)
                    if lz:
                        S.pool(lambda e, p_=p_: e.memset(p_[:, 0:2], 0.0), reads=[pk], writes=[pk])
                    if rz:
                        S.pool(lambda e, p_=p_, N=N: e.memset(p_[:, 2 + N:4 + N], 0.0), reads=[pk], writes=[pk])
                    pos = lo
                    bi = 0
                    while pos < hi:
                        n_ = min(512, hi - pos)
                        bank = 1 + (bi % 2)
                        bi += 1
                        for kk in range(8):
                            S.pe(lambda e, bank=bank, kk=kk, pos=pos, n_=n_, w_=w_: e.matmul(B[bank][:, 0:n_], lhsT=w_[:, kk, :], rhs=aT_all[:, kk, pos:pos + n_], start=(kk == 0), stop=(kk == 7)),
                                 reads=aTkeys + [wk], writes=[f"B{bank}"])
                        dst0 = pos - c0 + 2
                        S.act(lambda e, bank=bank, p_=p_, dst0=dst0, n_=n_: e.copy(out=p_[:, dst0:dst0 + n_], in_=B[bank][:, 0:n_]), reads=[f"B{bank}", pk], writes=[pk])
                        pos += n_
                    S.dve(lambda e, a_=a_, p_=p_, N=N, ch=ch: e.tensor_scalar(out=a_[:, 0:N], in0=p_[:, 0:N], scalar1=cw[:, ch, 0:1], scalar2=None, op0=ALU.mult), reads=[pk, "cw"], writes=[ak])
                    for j in range(1, 5):
                        S.dve(lambda e, a_=a_, p_=p_, N=N, ch=ch, j=j: e.scalar_tensor_tensor(out=a_[:, 0:N], in0=p_[:, j:j + N], scalar=cw[:, ch, j:j + 1], in1=a_[:, 0:N], op0=ALU.mult, op1=ALU.add),
                              reads=[pk, "cw", ak], writes=[ak])
                    S.act(lambda e, a_=a_, N=N: e.activation(out=a_[:, 0:N], in_=a_[:, 0:N], func=AF.Silu), reads=[ak], writes=[ak])
                    if ch < 16:
                        S.act(lambda e, a_=a_, N=N: e.activation(out=sq[:, 0:N], in_=a_[:, 0:N], func=AF.Square), reads=[ak], writes=["sq"])
                        sc = (128.0 ** -0.5) if ch < 8 else 1.0
                        for j0 in range(0, N, 512):
                            n_ = min(512, N - j0)
                            S.pe(lambda e, j0=j0, n_=n_: e.matmul(B[3][:, 0:n_], lhsT=onesb2[:], rhs=sq[:, j0:j0 + n_], start=True, stop=True), reads=["onesb2", "sq"], writes=["B3"])
                            S.act(lambda e, n_=n_: e.activation(out=rs[:, 0:n_], in_=B[3][:, 0:n_], func=AF.Ln, bias=k.eps[:], scale=1.0), reads=["B3", "eps"], writes=["rs"])
                            S.act(lambda e, n_=n_: e.activation(out=rs[:, 0:n_], in_=rs[:, 0:n_], func=AF.Exp, scale=-0.5), reads=["rs"], writes=["rs"])
                            S.dve(lambda e, a_=a_, o_=o_, j0=j0, n_=n_, sc=sc: e.scalar_tensor_tensor(out=o_[:, j0:j0 + n_], in0=a_[:, j0:j0 + n_], scalar=sc, in1=rs[:, 0:n_], op0=ALU.mult, op1=ALU.mult),
                                  reads=[ak, "rs", ok], writes=[ok])
                    else:
                        S.dve(lambda e, a_=a_, o_=o_, N=N: e.tensor_copy(out=o_[:, 0:N], in_=a_[:, 0:N]), reads=[ak], writes=[ok])
                    S.dma("sp", k.fm[ch, :, dcol:dcol + N], o_[:, 0:N], reads=[ok], writes=["fm"])
            S.barrier(k.bar[:])
        S.barrier(k.bar[:])


def phase_dn_scan(k, which):
    S, nc = k.S, k.nc
    B, BB = k.banks, k.bbf
    A = which == "A"
    C = DN_C
    pf = "d" + which + "_"
    if A:
        chunks = list(range(NTOKP // C))
    else:
        chunks = list(range(NTOKP // C - 1, LCTX // C - 1, -1))
    gcol = 0 if A else 8
    with ExitStack() as stp:
        TT = lambda name, shape, dt: stp.enter_context(nc.sbuf_tensor(pf + name, list(shape), dt))
        dc = TT("dc", [64, 128 + 512 * 4], F32)
        S.dma("sp", dc[:], k.dnconst[:, (0 if A else 2176):(0 if A else 2176) + 2176], writes=["dc"])
        idb = TT("idb", [128, 128], BF16)
        ones = TT("ones", [64, 128], F32)
        S.dma("pool", idb[:], k.consts[:, 0:128], writes=["idb"])
        S.pool(lambda e: e.memset(ones[:], 1.0), writes=["ones"])
        tri = dc[:, 0:64]
        strict = dc[:, 128:640]
        inclT = dc[:, 640:1152]
        bdiag = dc[:, 1152:1664]
        idrep = dc[:, 1664:2176]
        id64b = idb[0:64, 0:64]
        Sf = TT("Sf", [128, 1024], F32)
        Sb = TT("Sb", [128, 1024], BF16)
        h3 = lambda ap: ap.rearrange("p (h d) -> p h d", h=8)
        if A:
            S.pool(lambda e: e.memset(Sf[:], 0.0), writes=["Sf"])
        else:
            S.dma("sp", h3(Sf[:]), k.sin.rearrange("(h p) v -> p h v", p=128), reads=["sin"], writes=["Sf"])
        S.act(lambda e: e.copy(out=Sb[:], in_=Sf[:]), reads=["Sf"], writes=["Sb"])

        def mk(name, shape, dt, n=2):
            return [TT(f"{name}{i}", shape, dt) for i in range(n)]
        kT8 = mk("kT8", [128, 512], BF16)
        qT8 = mk("qT8", [128, 512], BF16)
        vT8 = mk("vT8", [128, 512], BF16)
        gbc = mk("gbc", [64, 32], F32)
        gc = TT("gc", [64, 8], F32)
        gl = mk("gl", [128, 8], F32)
        sm = TT("sm", [64, 32], F32)
        dg = TT("dg", [64, 512], F32)
        egw = TT("egw", [128, 512], F32)
        E1 = TT("E1", [64, 512], F32)
        Dn = TT("Dn", [64, 512], F32)
        Dp = TT("Dp", [64, 512], F32)
        ktm = TT("ktm", [64, 1024], BF16)
        Lm = TT("Lm", [64, 512], F32)
        OL = TT("OL", [64, 512], BF16)
        XT = mk("XT", [64, 512], BF16)
        X = mk("X", [64, 512], BF16)
        Pm = TT("Pm", [64, 512], F32)
        Pb = TT("Pb", [64, 512], BF16)
        MT = TT("MT", [64, 512], BF16)
        T1 = TT("T1", [64, 512], F32)
        T1b = TT("T1b", [64, 512], BF16)
        Ai = TT("Ai", [64, 512], BF16)
        bv = TT("bv", [64, 1024], BF16)
        kbg = TT("kbg", [64, 1024], BF16)
        qgT = mk("qgT", [128, 512], BF16)
        qkT = mk("qkT", [64, 512], BF16)
        kd = mk("kd", [64, 1024], BF16)
        us = mk("us", [64, 1024], F32)
        wT = mk("wT", [128, 512], BF16)
        vn = TT("vn", [64, 1024], BF16)
        ot = mk("ot", [64, 1024], F32)

        def prep(ci, sl):
            col = ci * C
            s = str(sl)
            S.dma("sp", h3(kT8[sl][:]), k.fm[8:16, :, col:col + C].rearrange("h p c -> p h c"), reads=["fm"], writes=["kT8" + s])
            S.dma("sp", h3(qT8[sl][:]), k.fm[0:8, :, col:col + C].rearrange("h p c -> p h c"), reads=["fm"], writes=["qT8" + s])
            S.dma("sp", h3(vT8[sl][:]), k.fm[16:24, :, col:col + C].rearrange("h p c -> p h c"), reads=["fm"], writes=["vT8" + s])
            S.dma("sp", gbc[sl][:], k.gb[col:col + C, :], reads=["gb"], writes=["gbc" + s])
            g8 = gbc[sl][:, gcol:gcol + 8]
            b8 = gbc[sl][:, 16 + gcol:16 + gcol + 8]
            S.pe(lambda e: e.matmul(B[1][0:64, 0:8], lhsT=tri, rhs=g8, start=True, stop=True), reads=["dc", "gbc" + s], writes=["B1"])
            S.pe(lambda e: e.matmul(B[1][:, 8:16], lhsT=ones[:], rhs=g8, start=True, stop=True), reads=["ones", "gbc" + s], writes=["B1"])
            S.dve(lambda e: e.tensor_copy(out=gc[:], in_=B[1][0:64, 0:8]), reads=["B1"], writes=["gc"])
            S.act(lambda e: e.activation(out=gl[sl][:], in_=B[1][:, 8:16], func=AF.Exp), reads=["B1"], writes=["gl" + s])
            S.act(lambda e: e.activation(out=sm[:, 0:8], in_=gc[:], func=AF.Exp), reads=["gc"], writes=["sm"])
            S.dve(lambda e: e.tensor_tensor(out=sm[:, 8:16], in0=sm[:, 0:8], in1=b8, op=ALU.mult), reads=["sm", "gbc" + s], writes=["sm"])
            S.dve(lambda e: e.tensor_tensor(out=sm[:, 16:24], in0=B[1][0:64, 8:16], in1=gc[:], op=ALU.subtract), reads=["B1", "gc", "sm"], writes=["sm"])
            S.act(lambda e: e.activation(out=sm[:, 16:24], in_=sm[:, 16:24], func=AF.Exp), reads=["sm"], writes=["sm"])
            S.dve(lambda e: e.tensor_tensor(out=h3(dg[:]), in0=h3(idrep), in1=bc_free(gc[:], [64, 8, 64]), op=ALU.mult), reads=["dc", "gc"], writes=["dg"])
            S.pe(lambda e: e.matmul(B[1][:], lhsT=ones[:], rhs=dg[:], start=True, stop=True), reads=["ones", "dg"], writes=["B1"])
            S.act(lambda e: e.activation(out=egw[:], in_=B[1][:], func=AF.Exp), reads=["B1"], writes=["egw"])
            S.dve(lambda e: e.tensor_tensor(out=qgT[sl][:], in0=qT8[sl][:], in1=egw[:], op=ALU.mult), reads=["qT8" + s, "egw"], writes=["qgT" + s])
            S.dve(lambda e: e.tensor_tensor(out=h3(E1[:]), in0=bc_free(gc[:], [64, 8, 64]), in1=h3(B[1][0:64, :]), op=ALU.subtract), reads=["gc", "B1"], writes=["E1"])
            S.dve(lambda e: e.tensor_scalar(out=Dn[:], in0=E1[:], scalar1=0.0, scalar2=None, op0=ALU.min), reads=["E1"], writes=["Dn"])
            S.dve(lambda e: e.tensor_scalar(out=Dp[:], in0=E1[:], scalar1=0.0, scalar2=-1.0, op0=ALU.max, op1=ALU.mult), reads=["E1"], writes=["Dp"])
            S.act(lambda e: e.activation(out=Dn[:], in_=Dn[:], func=AF.Exp), reads=["Dn"], writes=["Dn"])
            S.act(lambda e: e.activation(out=Dp[:], in_=Dp[:], func=AF.Exp), reads=["Dp"], writes=["Dp"])
            for h in range(8):
                S.pe(lambda e, h=h: e.transpose(out=BB[2][0:64, h * 128:(h + 1) * 128], in_=kT8[sl][:, h * 64:(h + 1) * 64], identity=idb[:]), reads=["kT8" + s, "idb"], writes=["B2"])
            for h in range(8):
                S.pe(lambda e, h=h: e.transpose(out=BB[3][0:64, h * 128:(h + 1) * 128], in_=vT8[sl][:, h * 64:(h + 1) * 64], identity=idb[:]), reads=["vT8" + s, "idb"], writes=["B3"])
            S.act(lambda e: e.copy(out=ktm[:], in_=BB[2][0:64, 0:1024]), reads=["B2"], writes=["ktm"])
            S.dve(lambda e: e.tensor_tensor(out=h3(bv[:]), in0=h3(BB[3][0:64, 0:1024]), in1=bc_free(b8, [64, 8, 128]), op=ALU.mult), reads=["B3", "gbc" + s], writes=["bv"])
            S.dve(lambda e: e.tensor_tensor(out=h3(kbg[:]), in0=h3(ktm[:]), in1=bc_free(sm[:, 8:16], [64, 8, 128]), op=ALU.mult), reads=["ktm", "sm"], writes=["kbg"])
            S.pool(lambda e: e.tensor_tensor(out=h3(kd[sl][:]), in0=h3(ktm[:]), in1=bc_free(sm[:, 16:24], [64, 8, 128]), op=ALU.mult), reads=["ktm", "sm"], writes=["kd" + s])
            for h in range(8):
                S.pe(lambda e, h=h: e.matmul(B[4][0:64, h * 64:(h + 1) * 64], lhsT=kT8[sl][:, h * 64:(h + 1) * 64], rhs=kT8[sl][:, h * 64:(h + 1) * 64], start=True, stop=True), reads=["kT8" + s], writes=["B4"])
            for h in range(8):
                S.pe(lambda e, h=h: e.matmul(B[5][0:64, h * 64:(h + 1) * 64], lhsT=kT8[sl][:, h * 64:(h + 1) * 64], rhs=qT8[sl][:, h * 64:(h + 1) * 64], start=True, stop=True), reads=["kT8" + s, "qT8" + s], writes=["B5"])
            S.dve(lambda e: e.tensor_tensor(out=Dn[:], in0=Dn[:], in1=strict, op=ALU.mult), reads=["Dn", "dc"], writes=["Dn"])
            S.dve(lambda e: e.tensor_tensor(out=h3(Dn[:]), in0=h3(Dn[:]), in1=bc_free(b8, [64, 8, 64]), op=ALU.mult), reads=["Dn", "gbc" + s], writes=["Dn"])
            S.dve(lambda e: e.tensor_tensor(out=Lm[:], in0=B[4][0:64, :], in1=Dn[:], op=ALU.mult), reads=["B4", "Dn"], writes=["Lm"])
            S.pool(lambda e: e.tensor_tensor(out=Dp[:], in0=Dp[:], in1=inclT, op=ALU.mult), reads=["Dp", "dc"], writes=["Dp"])
            S.dve(lambda e: e.tensor_tensor(out=qkT[sl][:], in0=B[5][0:64, :], in1=Dp[:], op=ALU.mult), reads=["B5", "Dp"], writes=["qkT" + s])
            S.dve(lambda e: e.tensor_tensor(out=T1[:], in0=Lm[:], in1=bdiag, op=ALU.mult), reads=["Lm", "dc"], writes=["T1"])
            S.dve(lambda e: e.tensor_tensor(out=OL[:], in0=Lm[:], in1=T1[:], op=ALU.subtract), reads=["Lm", "T1"], writes=["OL"])
            S.dve(lambda e: e.tensor_scalar(out=XT[0][:], in0=T1[:], scalar1=-1.0, scalar2=None, op0=ALU.mult), reads=["T1"], writes=["XT0"])
            for h in range(8):
                S.pe(lambda e, h=h: e.matmul(B[4][0:64, h * 64:(h + 1) * 64], lhsT=XT[0][:, h * 64:(h + 1) * 64], rhs=id64b, start=True, stop=True), reads=["XT0", "idb"], writes=["B4"])
            S.act(lambda e: e.copy(out=X[0][:], in_=B[4][0:64, :]), reads=["B4"], writes=["X0"])
            S.dve(lambda e: e.tensor_tensor(out=Pm[:], in0=B[4][0:64, :], in1=idrep, op=ALU.add), reads=["B4", "dc", "X0"], writes=["Pm"])
            S.act(lambda e: e.copy(out=Pb[:], in_=Pm[:]), reads=["Pm"], writes=["Pb"])
            cur = 0
            for i in range(4):
                nx = 1 - cur
                for h in range(8):
                    S.pe(lambda e, h=h, cur=cur: e.matmul(B[5][0:64, h * 64:(h + 1) * 64], lhsT=X[cur][:, h * 64:(h + 1) * 64], rhs=XT[cur][:, h * 64:(h + 1) * 64], start=True, stop=True),
                         reads=[f"X{cur}", f"XT{cur}"], writes=["B5"])
                if i < 3:
                    for h in range(8):
                        S.pe(lambda e, h=h, cur=cur: e.matmul(B[4][0:64, h * 64:(h + 1) * 64], lhsT=XT[cur][:, h * 64:(h + 1) * 64], rhs=X[cur][:, h * 64:(h + 1) * 64], start=True, stop=True),
                             reads=[f"X{cur}", f"XT{cur}"], writes=["B4"])
                S.act(lambda e, nx=nx: e.copy(out=XT[nx][:], in_=B[5][0:64, :]), reads=["B5"], writes=[f"XT{nx}"])
                if i < 3:
                    S.dve(lambda e, nx=nx: e.tensor_copy(out=X[nx][:], in_=B[4][0:64, :]), reads=["B4"], writes=[f"X{nx}"])
                for h in range(8):
                    S.pe(lambda e, h=h, nx=nx: e.matmul(B[2][0:64, h * 64:(h + 1) * 64], lhsT=XT[nx][:, h * 64:(h + 1) * 64], rhs=Pb[:, h * 64:(h + 1) * 64], start=True, stop=True),
                         reads=[f"XT{nx}", "Pb"], writes=["B2"])
                S.dve(lambda e: e.tensor_tensor(out=Pm[:], in0=Pm[:], in1=B[2][0:64, 0:512], op=ALU.add), reads=["Pm", "B2"], writes=["Pm"])
                S.act(lambda e: e.copy(out=Pb[:], in_=Pm[:]), reads=["Pm"], writes=["Pb"])
                cur = nx
            for h in range(8):
                S.pe(lambda e, h=h: e.matmul(B[3][0:64, h * 64:(h + 1) * 64], lhsT=Pb[:, h * 64:(h + 1) * 64], rhs=id64b, start=True, stop=True), reads=["Pb", "idb"], writes=["B3"])
            S.act(lambda e: e.copy(out=MT[:], in_=B[3][0:64, :]), reads=["B3"], writes=["MT"])
            for h in range(8):
                S.pe(lambda e, h=h: e.matmul(B[4][0:64, h * 64:(h + 1) * 64], lhsT=OL[:, h * 64:(h + 1) * 64], rhs=Pb[:, h * 64:(h + 1) * 64], start=True, stop=True), reads=["OL", "Pb"], writes=["B4"])
            S.act(lambda e: e.copy(out=T1b[:], in_=B[4][0:64, :]), reads=["B4"], writes=["T1b"])
            for h in range(8):
                S.pe(lambda e, h=h: e.matmul(B[5][0:64, h * 64:(h + 1) * 64], lhsT=MT[:, h * 64:(h + 1) * 64], rhs=T1b[:, h * 64:(h + 1) * 64], start=True, stop=True), reads=["MT", "T1b"], writes=["B5"])
            S.dve(lambda e: e.tensor_tensor(out=Ai[:], in0=Pm[:], in1=B[5][0:64, :], op=ALU.subtract), reads=["Pm", "B5"], writes=["Ai"])
            for h in range(8):
                S.pe(lambda e, h=h: e.matmul(B[2 + h // 4][0:64, (h % 4) * 128:(h % 4 + 1) * 128], lhsT=Ai[:, h * 64:(h + 1) * 64], rhs=bv[:, h * 128:(h + 1) * 128], start=True, stop=True),
                     reads=["Ai", "bv"], writes=[f"B{2 + h // 4}"])
            S.act(lambda e: e.copy(out=us[sl][:, 0:512], in_=B[2][0:64, :]), reads=["B2"], writes=["us" + s])
            S.dve(lambda e: e.tensor_copy(out=us[sl][:, 512:1024], in_=B[3][0:64, :]), reads=["B3", "us" + s], writes=["us" + s])
            for h in range(8):
                S.pe(lambda e, h=h: e.matmul(B[4][:, h * 64:(h + 1) * 64], lhsT=kbg[:, h * 128:(h + 1) * 128], rhs=Ai[:, h * 64:(h + 1) * 64], start=True, stop=True), reads=["kbg", "Ai"], writes=["B4"])
            S.act(lambda e: e.copy(out=wT[sl][:], in_=B[4][:]), reads=["B4"], writes=["wT" + s])

        def scan(ci, sl, odst):
            s = str(sl)
            for h in range(8):
                S.pe(lambda e, h=h: e.matmul(B[6 + h // 4][0:64, (h % 4) * 128:(h % 4 + 1) * 128], lhsT=wT[sl][:, h * 64:(h + 1) * 64], rhs=Sb[:, h * 128:(h + 1) * 128], start=True, stop=True),
                     reads=["wT" + s, "Sb"], writes=[f"B{6 + h // 4}"])
            S.dve(lambda e: e.tensor_tensor(out=vn[:, 0:512], in0=us[sl][:, 0:512], in1=B[6][0:64, :], op=ALU.subtract), reads=["us" + s, "B6"], writes=["vn"])
            S.dve(lambda e: e.tensor_tensor(out=vn[:, 512:1024], in0=us[sl][:, 512:1024], in1=B[7][0:64, :], op=ALU.subtract), reads=["us" + s, "B7", "vn"], writes=["vn"])
            if odst is not None:
                for h in range(8):
                    bank = 6 + h // 4
                    S.pe(lambda e, h=h, bank=bank: e.matmul(B[bank][0:64, (h % 4) * 128:(h % 4 + 1) * 128], lhsT=qgT[sl][:, h * 64:(h + 1) * 64], rhs=Sb[:, h * 128:(h + 1) * 128], start=True, stop=False),
                         reads=["qgT" + s, "Sb"], writes=[f"B{bank}"])
                    S.pe(lambda e, h=h, bank=bank: e.matmul(B[bank][0:64, (h % 4) * 128:(h % 4 + 1) * 128], lhsT=qkT[sl][:, h * 64:(h + 1) * 64], rhs=vn[:, h * 128:(h + 1) * 128], start=False, stop=True),
                         reads=["qkT" + s, "vn"], writes=[f"B{bank}"])
                o_ = ot[ci % 2]
                ok = f"ot{ci % 2}"
                S.act(lambda e, o_=o_: e.copy(out=o_[:, 0:512], in_=B[6][0:64, :]), reads=["B6", ok], writes=[ok])
                S.act(lambda e, o_=o_: e.copy(out=o_[:, 512:1024], in_=B[7][0:64, :]), reads=["B7", ok], writes=[ok])
                S.dma("sp", odst, o_[:], reads=[ok], writes=["o" + which])
            S.dve(lambda e: e.tensor_tensor(out=h3(Sf[:]), in0=h3(Sf[:]), in1=bc_free(gl[sl][:], [128, 8, 128]), op=ALU.mult), reads=["Sf", "gl" + s], writes=["Sf"])
            for hh in range(2):
                for h in range(hh * 4, hh * 4 + 4):
                    S.pe(lambda e, h=h: e.matmul(B[0][:, (h % 4) * 128:(h % 4 + 1) * 128], lhsT=kd[sl][:, h * 128:(h + 1) * 128], rhs=vn[:, h * 128:(h + 1) * 128], start=True, stop=True),
                         reads=["kd" + s, "vn"], writes=["B0"])
                S.dve(lambda e, hh=hh: e.tensor_tensor(out=Sf[:, hh * 512:(hh + 1) * 512], in0=Sf[:, hh * 512:(hh + 1) * 512], in1=B[0][:], op=ALU.add), reads=["Sf", "B0"], writes=["Sf"])
            S.act(lambda e: e.copy(out=Sb[:], in_=Sf[:]), reads=["Sf"], writes=["Sb"])

        odram = k.oA if A else k.oB
        prep(chunks[0], 0)
        for idx, ci in enumerate(chunks):
            if idx + 1 < len(chunks):
                prep(chunks[idx + 1], (idx + 1) % 2)
            lat = ci - LCTX // C
            odst = odram[lat * C:(lat + 1) * C, :] if lat >= 0 else None
            scan(ci, idx % 2, odst)
        if A:
            S.dma("sp", k.sout.rearrange("(h p) v -> p h v", p=128), h3(Sf[:]), reads=["Sf"], writes=["sout"])
        S.barrier(k.bar[:])


def phase_dn_exchange(k):
    S, nc = k.S, k.nc
    S.op("pool", lambda e: e.collective_compute("AllGather", ALU.bypass, ins=[k.sout], outs=[k.sgat],
                                                 replica_groups=[[0, 1], [2, 3], [4, 5], [6, 7]]),
         reads=["sout"], writes=["sgat"], dma=True)
    with ExitStack() as stp:
        TT = lambda name, shape, dt: stp.enter_context(nc.sbuf_tensor("dx_" + name, list(shape), dt))
        g0 = TT("g0", [128, 8, 128], F32)
        g1 = TT("g1", [128, 8, 128], F32)
        sel = TT("sel", [128, 2], F32)
        S.dma("sp", sel[:], k.selv, writes=["sel"])
        S.dma("sp", g0[:], k.sgat[0:1024, :].rearrange("(h p) v -> p h v", p=128), reads=["sgat"], writes=["g0"])
        S.dma("sp", g1[:], k.sgat[1024:2048, :].rearrange("(h p) v -> p h v", p=128), reads=["sgat"], writes=["g1"])
        S.dve(lambda e: e.tensor_scalar(out=g0[:], in0=g0[:], scalar1=sel[:, 0:1], scalar2=None, op0=ALU.mult), reads=["g0", "sel"], writes=["g0"])
        S.dve(lambda e: e.scalar_tensor_tensor(out=g0[:], in0=g1[:], scalar=sel[:, 1:2], in1=g0[:], op0=ALU.mult, op1=ALU.add), reads=["g0", "g1", "sel"], writes=["g0"])
        S.dma("sp", k.sin.rearrange("(h p) v -> p h v", p=128), g0[:], reads=["g0"], writes=["sin"])
        S.barrier(k.bar[:])


def phase_dn_out(k):
    S, nc = k.S, k.nc
    B, BB = k.banks, k.bbf
    modl = k.modl
    with ExitStack() as stp:
        TT = lambda name, shape, dt: stp.enter_context(nc.sbuf_tensor("do_" + name, list(shape), dt))
        idb = TT("idb", [128, 128], BF16)
        wo = TT("wo", [128, 8, 1024], BF16)
        og = TT("og", [128, 128], F32)
        S.dma("pool", idb[:], k.consts[:, 0:128], writes=["idb"])
        S.dma("pool", wo[:], k.dn_w_o.rearrange("(k p) n -> p k n", p=128), writes=["wo"])
        S.dma("sp", og[:], k.dn_ogain.partition_broadcast(128), writes=["og"])
        oa = [TT(f"oa{i}", [128, D], F32) for i in range(2)]
        ob = [TT(f"ob{i}", [128, D], F32) for i in range(2)]
        zt = [TT(f"zt{i}", [128, D], BF16) for i in range(2)]
        hx = [TT(f"hx{i}", [128, D], F32) for i in range(2)]
        sq = TT("sq", [128, D], F32)
        ssq = TT("ssq", [128, 8], F32)
        rs = TT("rs", [128, 8], F32)
        yb = TT("yb", [128, D], BF16)
        yT = TT("yT", [128, D], BF16)
        h8 = lambda ap: ap.rearrange("p (h d) -> p h d", h=8)
        for n in range(32):
            a, b_, z_, h_ = oa[n % 2], ob[n % 2], zt[n % 2], hx[n % 2]
            ak, bk, zk, hk = f"oa{n % 2}", f"ob{n % 2}", f"ozt{n % 2}", f"ohx{n % 2}"
            rows = slice(n * 128, (n + 1) * 128)
            S.dma("sp", a[:], k.oA[rows, :], reads=["oA"], writes=[ak])
            S.dma("act", b_[:], k.oB[rows, :], reads=["oB"], writes=[bk])
            S.dma("sp", z_[:], k.zs[rows, :], reads=["zs"], writes=[zk])
            S.dma("act", h_[:], k.h2[rows, :], reads=["h2"], writes=[hk])
            S.dve(lambda e, a=a, b_=b_: e.tensor_tensor(out=a[:], in0=a[:], in1=b_[:], op=ALU.add), reads=[ak, bk], writes=[ak])
            S.act(lambda e, a=a: e.activation(out=sq[:], in_=a[:], func=AF.Square), reads=[ak], writes=["sq"])
            S.dve(lambda e: e.tensor_reduce(out=ssq[:], in_=h8(sq[:]), axis=AX.X, op=ALU.add), reads=["sq"], writes=["ssq"])
            rstd_from_ssq(k, ssq[:], rs[:], 1.0 / 128, "ssq", "rs")
            S.dve(lambda e, a=a: e.tensor_tensor(out=h8(a[:]), in0=h8(a[:]), in1=bc_free(rs[:], [128, 8, 128]), op=ALU.mult), reads=[ak, "rs"], writes=[ak])
            S.pool(lambda e, a=a: e.tensor_tensor(out=h8(a[:]), in0=h8(a[:]), in1=bc_mid(og[:], [128, 8, 128]), op=ALU.mult), reads=[ak, "og"], writes=[ak])
            S.dve(lambda e, a=a, z_=z_: e.tensor_tensor(out=yb[:], in0=a[:], in1=z_[:], op=ALU.mult), reads=[ak, zk], writes=["yb"])
            for kk in range(8):
                S.pe(lambda e, kk=kk: e.transpose(out=BB[0][:, kk * 128:(kk + 1) * 128], in_=yb[:, kk * 128:(kk + 1) * 128], identity=idb[:]), reads=["yb", "idb"], writes=["B0"])
            S.act(lambda e: e.copy(out=yT[:], in_=BB[0][:, 0:D]), reads=["B0"], writes=["yT"])
            for half in range(2):
                for kk in range(8):
                    S.pe(lambda e, half=half, kk=kk: e.matmul(B[1 + half][:], lhsT=yT[:, kk * 128:(kk + 1) * 128], rhs=wo[:, kk, half * 512:(half + 1) * 512], start=(kk == 0), stop=(kk == 7)),
                         reads=["yT", "wo"], writes=[f"B{1 + half}"])
                S.dve(lambda e, half=half, a=a: e.tensor_tensor(out=a[:, half * 512:(half + 1) * 512], in0=B[1 + half][:], in1=modl[:, 2 * D + half * 512:2 * D + (half + 1) * 512], op=ALU.mult),
                      reads=[f"B{1 + half}", ak], writes=[ak])
            S.dve(lambda e, a=a, h_=h_: e.tensor_tensor(out=h_[:], in0=h_[:], in1=a[:], op=ALU.add), reads=[ak, hk], writes=[hk])
            S.dma("sp", k.h3[rows, :], h_[:], reads=[hk], writes=["h3"])
        S.barrier(k.bar[:])


def rope_tables(times):
    t = np.asarray(times)
    row = (t // 64).astype(np.float32)
    col = (t % 64).astype(np.float32)
    inv = (np.float32(10000.0) ** (-np.arange(16, dtype=np.float32) / np.float32(16))).astype(np.float32)
    out_c = np.zeros((len(t), 64), np.float32)
    out_s = np.zeros((len(t), 64), np.float32)
    for a, pos in enumerate((row, col)):
        ang = (pos[:, None] * inv[None, :]).astype(np.float32)
        c, s = np.cos(ang).astype(np.float32), np.sin(ang).astype(np.float32)
        out_c[:, a * 32:a * 32 + 16] = c
        out_c[:, a * 32 + 16:a * 32 + 32] = c
        out_s[:, a * 32:a * 32 + 16] = -s
        out_s[:, a * 32 + 16:a * 32 + 32] = s
    return out_c, out_s


def make_consts():
    c = np.zeros((128, 1280), np.float32)
    c[:, 0:128] = np.eye(128, dtype=np.float32)
    j = np.arange(128)[:, None]
    i = np.arange(128)[None, :]
    c[:, 128:256] = (j < i).astype(np.float32)
    mp = np.where(j >= i, 0.0, NEG).astype(np.float32)
    mn = np.where(j <= i, 0.0, NEG).astype(np.float32)
    c[:, 256:768] = np.tile(mp, (1, 4))
    c[:, 768:1280] = np.tile(mn, (1, 4))
    return c


def make_mconst():
    m = np.zeros((128, 96), np.float32)
    m[:, 0:16] = (np.arange(16) * MB)[None, :]
    p = np.arange(128)[:, None]
    m[:, 16:24] = np.arange(8)[None, :] * 128 + p
    m[:, 24:28] = np.arange(4)[None, :] * 128 + p
    m[:, 32:96] = np.arange(64)[None, :]
    return m


def make_dnconst():
    out = np.zeros((64, 2 * 2176), np.float32)
    i = np.arange(64)[:, None]
    j = np.arange(64)[None, :]
    for d in range(2):
        A = d == 0
        o = d * 2176
        out[:, o:o + 64] = ((i <= j) if A else (i >= j)).astype(np.float32)
        out[:, o + 64:o + 128] = np.eye(64, dtype=np.float32)
        strict = ((j < i) if A else (j > i)).astype(np.float32)
        inclT = ((i <= j) if A else (i >= j)).astype(np.float32)
        bd = ((i // 32) == (j // 32)).astype(np.float32)
        out[:, o + 128:o + 640] = np.tile(strict, (1, 8))
        out[:, o + 640:o + 1152] = np.tile(inclT, (1, 8))
        out[:, o + 1152:o + 1664] = np.tile(bd, (1, 8))
        out[:, o + 1664:o + 2176] = np.tile(np.eye(64, dtype=np.float32), (1, 8))
    return out


def dn_inputs(inputs, half):
    f = lambda a: np.ascontiguousarray(np.asarray(a, dtype=np.float32))
    w_in = f(inputs["dn_w_in"][0]).copy()
    conv = f(inputs["dn_conv_w"][0])
    alog = f(inputs["dn_a_log"][0])
    dtb = f(inputs["dn_dt_bias"][0])
    if half == 1:
        ab = w_in[:, 4096:4128].reshape(D, 2, 2, 8)[:, :, ::-1, :]
        w_in[:, 4096:4128] = ab.reshape(D, 32)
        conv = conv[::-1]
        alog = alog[::-1]
        dtb = dtb[::-1]
    sel = np.zeros((128, 2), np.float32)
    sel[:, 1 - half] = 1.0
    return {
        "dn_w_in": np.ascontiguousarray(w_in),
        "dn_conv": np.ascontiguousarray(conv.T.reshape(24, 128, 5).transpose(1, 0, 2).reshape(128, 120)),
        "dn_alog": np.ascontiguousarray(alog.reshape(16)),
        "dn_dtb": np.ascontiguousarray(dtb.reshape(16)),
        "dn_ogain": f(inputs["dn_o_gain"][0]),
        "dn_w_o": f(inputs["dn_w_o"][0]),
        "dnconst": make_dnconst(),
        "selv": sel,
    }


def core_times(half):
    s = np.arange(NLAT)
    return s if half == 0 else (8191 - s)


def make_in_maps(inputs, mode="full"):
    f = lambda a: np.ascontiguousarray(np.asarray(a, dtype=np.float32))
    x, c, ctx, c_ctx = f(inputs["x"]), f(inputs["c"]), f(inputs["ctx"]), f(inputs["c_ctx"])
    lsel = {"full": slice(0, 2), "L1": slice(0, 1), "L2": slice(1, 2)}[mode]
    shared = {
        "consts": make_consts(),
        "ada_w": f(inputs["ada_w"]),
        "ada_b": f(inputs["ada_b"]),
        "norm_g": f(inputs["norm_g"]).reshape(2, 2 * D),
        "mconst": make_mconst(),
        "w_router": np.ascontiguousarray(np.concatenate([f(inputs["moe_w_group"]), f(inputs["moe_w_expert"])], axis=2)),
        "b_router": np.ascontiguousarray(np.concatenate([f(inputs["moe_b_group"]), f(inputs["moe_b_expert"])], axis=1)),
        "moe_w_in": np.ascontiguousarray(f(inputs["moe_w_in"])[lsel]),
        "moe_w_out": np.ascontiguousarray(f(inputs["moe_w_out"])[lsel]),
    }
    if mode in ("full", "L1"):
        shared.update({
            "attn_w_qkv": f(inputs["attn_w_qkv"][0]),
            "attn_w_o": f(inputs["attn_w_o"][0]),
            "qk_gain": np.concatenate([f(inputs["attn_q_gain"][0]), f(inputs["attn_k_gain"][0])]),
            "attn_sink": f(inputs["attn_sink"][0]),
        })
    maps = []
    for r in range(8):
        b, half = r // 2, r % 2
        m = dict(shared)
        cv = np.zeros((128, 16), np.float32)
        cv[:, 0:8] = c[b].reshape(8, 128).T
        cv[:, 8:16] = c_ctx.reshape(8, 128).T
        m["cvec"] = cv
        dn = dn_inputs(inputs, half)
        if mode in ("full", "L1"):
            tm = core_times(half)
            cosT, sinT = rope_tables(tm)
            m["x"] = np.ascontiguousarray(x[b][tm])
            m["ctx"] = np.ascontiguousarray(ctx[b] if half == 0 else ctx[b][::-1])
            m["cosT"], m["sinT"] = cosT, sinT
        keys = {"full": list(dn.keys()), "L1": ["dn_w_in", "dn_conv", "dn_alog", "dn_dtb", "dnconst"], "L2": ["dn_ogain", "dn_w_o", "dnconst"]}[mode]
        for kk in keys:
            m[kk] = dn[kk]
        maps.append(m)
    return maps


def assemble(results, key="out", rows=NOWN):
    out = np.zeros((4, 8192, D), np.float32)
    for r in range(8):
        b, half = r // 2, r % 2
        o = np.asarray(results[r][key])[:rows]
        tm = core_times(half)[:rows]
        out[b, tm] = o
    return out


def kernel(**inputs):
    nc1 = build(mode="L1")
    maps1 = make_in_maps(inputs, "L1")
    res1 = run_bass_kernel_spmd(nc1, maps1, core_ids=list(range(8))).results
    del maps1
    nc2 = build(mode="L2")
    maps2 = make_in_maps(inputs, "L2")
    for r in range(8):
        for kk in ("h2", "zs", "gb", "fm", "oA"):
            maps2[r][kk] = np.ascontiguousarray(res1[r][kk])
        maps2[r]["sin"] = np.ascontiguousarray(res1[r ^ 1]["sout"])
    res2 = run_bass_kernel_spmd(nc2, maps2, core_ids=list(range(8))).results
    return assemble(res2)
```
